# Optimizing a Trainium2 kernel written in Bass

```python
import math
import jax, jax.numpy as jnp
from jax import lax
import numpy as np

D_MODEL = 2048
BATCH = 4
SEQ = 4096
DEPTH = 1

N_META = 16
BLOCK = 128
PAD = BLOCK - N_META
HEAD_DIM = 128
N_FOX_HEADS = 8
N_RET_HEADS = 8
FOX_WIDTH = N_FOX_HEADS * HEAD_DIM
RET_WIDTH = N_RET_HEADS * HEAD_DIM
MIX_WIDTH = FOX_WIDTH + RET_WIDTH
SPLITS = (FOX_WIDTH, FOX_WIDTH, FOX_WIDTH, N_FOX_HEADS, RET_WIDTH, RET_WIDTH, RET_WIDTH, RET_WIDTH)
IN_COLS = sum(SPLITS)
RET_ROPE_BASE = 10000.0
N_GROUPS = 4
EXPERTS_PER_GROUP = 4
N_EXPERTS = N_GROUPS * EXPERTS_PER_GROUP
TOP_K = 2
D_EXPERT = 1024
EPS = 1e-6
MASK_VALUE = -1e30

kernel_name = "hymba_fox_retnet_hmoe"


def rmsnorm(x, g):
    xf = x.astype(jnp.float32)
    y = xf * lax.rsqrt(jnp.mean(xf * xf, axis=-1, keepdims=True) + EPS)
    return (y * g.astype(jnp.float32)).astype(x.dtype)


def to_heads(t, n_heads):
    b, p, _ = t.shape
    return t.reshape(b, p, n_heads, HEAD_DIM).transpose(0, 2, 1, 3)


def rotate_every_two(x):
    x1 = x[..., ::2]
    x2 = x[..., 1::2]
    return jnp.stack((-x2, x1), axis=-1).reshape(x.shape)


def forgetting_attention(q, k, v, log_f, key_valid):
    b, h, p, dh = q.shape
    nblk = p // BLOCK
    cum = jnp.cumsum(log_f, axis=-1)
    scale = HEAD_DIM ** -0.5
    kpos = jnp.arange(p)
    qb = q.reshape(b, h, nblk, BLOCK, dh).transpose(2, 0, 1, 3, 4)
    cb = cum.reshape(b, h, nblk, BLOCK).transpose(2, 0, 1, 3)

    def one_block(args):
        i, q_i, c_i = args
        s = jnp.einsum('bhqd,bhkd->bhqk', q_i, k).astype(jnp.float32) * scale
        s = s + c_i[..., :, None] - cum[:, :, None, :]
        qpos = i * BLOCK + jnp.arange(BLOCK)
        mask = (kpos[None, :] <= qpos[:, None]) & key_valid[None, :]
        s = jnp.where(mask[None, None], s, MASK_VALUE)
        pr = jax.nn.softmax(s, axis=-1)
        return jnp.einsum('bhqk,bhkd->bhqd', pr.astype(v.dtype), v)

    out = lax.map(one_block, (jnp.arange(nblk), qb, cb))
    return out.transpose(1, 2, 0, 3, 4).reshape(b, h, p, dh)


def retention_chunkwise(q, k, v, log_gamma):
    b, h, p, dh = q.shape
    n = p // BLOCK
    dt = q.dtype
    qc = q.reshape(b, h, n, BLOCK, dh)
    kc = k.reshape(b, h, n, BLOCK, dh)
    vc = v.reshape(b, h, n, BLOCK, dh)
    idx = jnp.arange(BLOCK, dtype=jnp.float32)
    lg = log_gamma[:, None]
    diff = idx[:, None] - idx[None, :]
    dmask = jnp.where(diff[None] >= 0, jnp.exp(lg[:, :, None] * jnp.maximum(diff, 0.0)[None]), 0.0)
    scores = jnp.einsum('bhnid,bhnjd->bhnij', qc, kc) * dmask[None, :, None].astype(dt)
    inner = jnp.einsum('bhnij,bhnje->bhnie', scores, vc)
    zeta = jnp.exp(lg * (BLOCK - 1 - idx)).astype(dt)
    kv = jnp.einsum('bhnjd,bhnje->bhnde', kc * zeta[None, :, None, :, None], vc)
    chunk_decay = jnp.exp(log_gamma * BLOCK).astype(dt)

    def step(state, kv_n):
        return state * chunk_decay[None, :, None, None] + kv_n, state

    _, prev = lax.scan(step, jnp.zeros((b, h, dh, dh), dt), kv.transpose(2, 0, 1, 3, 4))
    prev = prev.transpose(1, 2, 0, 3, 4)
    xi = jnp.exp(lg * (idx + 1.0)).astype(dt)
    cross = jnp.einsum('bhnid,bhnde->bhnie', qc * xi[None, :, None, :, None], prev)
    return (inner + cross).reshape(b, h, p, dh)


def hierarchical_moe(hn, w_rg, b_rg, w_re, b_re, w_gate, w_up, w_down):
    b, p, d = hn.shape
    t = hn.reshape(b * p, d)
    gl = (t @ w_rg).astype(jnp.float32) + b_rg.astype(jnp.float32)
    gp = jax.nn.softmax(gl, axis=-1)
    _, g_star = lax.top_k(gl, 1)
    p_group = jnp.take_along_axis(gp, g_star, axis=1)[:, 0]
    el = ((t @ w_re).astype(jnp.float32) + b_re.astype(jnp.float32)).reshape(-1, N_GROUPS, EXPERTS_PER_GROUP)
    el_sel = jnp.take_along_axis(el, g_star[:, :, None], axis=1)[:, 0]
    top_v, top_i = lax.top_k(el_sel, TOP_K)
    w_local = jax.nn.softmax(top_v, axis=-1) * p_group[:, None]
    gidx = g_star * EXPERTS_PER_GROUP + top_i
    combine = jnp.sum(jax.nn.one_hot(gidx, N_EXPERTS, dtype=jnp.float32) * w_local[..., None], axis=1)
    combine = combine.astype(t.dtype)
    y = jnp.zeros_like(t)
    for e in range(N_EXPERTS):
        a = jax.nn.silu(t @ w_gate[e]) * (t @ w_up[e])
        y = y + combine[:, e:e + 1] * (a @ w_down[e])
    return y.reshape(b, p, d)


def setup_inputs(seed: int = 0) -> dict:
    key = jax.random.key(seed)
    ks = jax.random.split(key, 20)
    f32 = jnp.float32
    nrm = lambda k, shape, s: jax.random.normal(k, shape, f32) * s
    gain = lambda k, shape: 1.0 + 0.02 * jax.random.normal(k, shape, f32)
    b_forget = jnp.linspace(1.0, 5.0, N_FOX_HEADS, dtype=f32)[None, :] + nrm(ks[4], (DEPTH, N_FOX_HEADS), 0.1)
    return {
        "x": nrm(ks[0], (BATCH, SEQ, D_MODEL), 1.0),
        "meta_tokens": nrm(ks[1], (N_META, D_MODEL), 1.0),
        "attn_norm_g": gain(ks[2], (DEPTH, D_MODEL)),
        "w_in": nrm(ks[3], (DEPTH, D_MODEL, IN_COLS), D_MODEL ** -0.5),
        "b_forget": b_forget,
        "fox_out_g": gain(ks[5], (DEPTH, FOX_WIDTH)),
        "ret_out_g": gain(ks[6], (DEPTH, RET_WIDTH)),
        "w_out": nrm(ks[7], (DEPTH, MIX_WIDTH, D_MODEL), MIX_WIDTH ** -0.5),
        "ffn_norm_g": gain(ks[8], (DEPTH, D_MODEL)),
        "w_router_group": nrm(ks[9], (DEPTH, D_MODEL, N_GROUPS), D_MODEL ** -0.5),
        "b_router_group": nrm(ks[10], (DEPTH, N_GROUPS), 0.01),
        "w_router_expert": nrm(ks[11], (DEPTH, D_MODEL, N_EXPERTS), D_MODEL ** -0.5),
        "b_router_expert": nrm(ks[12], (DEPTH, N_EXPERTS), 0.01),
        "w_gate": nrm(ks[13], (DEPTH, N_EXPERTS, D_MODEL, D_EXPERT), D_MODEL ** -0.5),
        "w_up": nrm(ks[14], (DEPTH, N_EXPERTS, D_MODEL, D_EXPERT), D_MODEL ** -0.5),
        "w_down": nrm(ks[15], (DEPTH, N_EXPERTS, D_EXPERT, D_MODEL), D_EXPERT ** -0.5),
        "final_norm_g": gain(ks[16], (D_MODEL,)),
    }


def reference(x, meta_tokens, attn_norm_g, w_in, b_forget, fox_out_g, ret_out_g, w_out,
              ffn_norm_g, w_router_group, b_router_group, w_router_expert, b_router_expert,
              w_gate, w_up, w_down, final_norm_g):
    b = x.shape[0]
    dt = x.dtype
    h = jnp.concatenate([
        jnp.zeros((b, PAD, D_MODEL), dt),
        jnp.broadcast_to(meta_tokens.astype(dt)[None], (b, N_META, D_MODEL)),
        x], axis=1)
    p = h.shape[1]
    pos = jnp.arange(p)
    valid = pos >= PAD
    log_gamma = jnp.log(1.0 - 2.0 ** (-5.0 - jnp.arange(N_RET_HEADS, dtype=jnp.float32)))
    angle = 1.0 / (RET_ROPE_BASE ** jnp.linspace(0.0, 1.0, HEAD_DIM // 2, dtype=jnp.float32))
    angle = jnp.repeat(angle, 2)
    phase = (pos - PAD).astype(jnp.float32)[:, None] * angle[None, :]
    sin = jnp.sin(phase).astype(dt)
    cos = jnp.cos(phase).astype(dt)
    offsets = list(np.cumsum(SPLITS)[:-1])

    for l in range(DEPTH):
        hn = rmsnorm(h, attn_norm_g[l])
        u = hn @ w_in[l]
        fq, fk, fv, flog, rq, rk, rv, rg = jnp.split(u, offsets, axis=-1)
        log_f = jax.nn.log_sigmoid(flog.astype(jnp.float32) + b_forget[l].astype(jnp.float32))
        log_f = log_f.transpose(0, 2, 1)
        o_fox = forgetting_attention(to_heads(fq, N_FOX_HEADS), to_heads(fk, N_FOX_HEADS),
                                     to_heads(fv, N_FOX_HEADS), log_f, valid)
        o_fox = rmsnorm(o_fox.transpose(0, 2, 1, 3).reshape(b, p, FOX_WIDTH), fox_out_g[l])
        qr = to_heads(rq, N_RET_HEADS)
        kr = to_heads(rk, N_RET_HEADS) * (HEAD_DIM ** -0.5)
        qr = qr * cos + rotate_every_two(qr) * sin
        kr = (kr * cos + rotate_every_two(kr) * sin) * valid[None, None, :, None].astype(dt)
        o_ret = retention_chunkwise(qr, kr, to_heads(rv, N_RET_HEADS), log_gamma)
        of = o_ret.astype(jnp.float32)
        mu = jnp.mean(of, axis=-1, keepdims=True)
        var = jnp.mean(jnp.square(of - mu), axis=-1, keepdims=True)
        of = (of - mu) * lax.rsqrt(var + EPS)
        of = of.transpose(0, 2, 1, 3).reshape(b, p, RET_WIDTH) * ret_out_g[l].astype(jnp.float32)
        o_ret = (of.astype(dt) * jax.nn.silu(rg))
        h = h + jnp.concatenate([o_fox, o_ret], axis=-1) @ w_out[l]
        hn2 = rmsnorm(h, ffn_norm_g[l])
        h = h + hierarchical_moe(hn2, w_router_group[l], b_router_group[l], w_router_expert[l],
                                 b_router_expert[l], w_gate[l], w_up[l], w_down[l])

    h = rmsnorm(h, final_norm_g)
    return h[:, BLOCK:, :]
```

```python
import numpy as np
import ml_dtypes
from contextlib import ExitStack
import concourse.bass as bass
import concourse.mybir as mybir
from concourse.bass_utils import run_bass_kernel_spmd

F32 = mybir.dt.float32
BF16 = mybir.dt.bfloat16
AF = mybir.ActivationFunctionType
ALU = mybir.AluOpType
AX = mybir.AxisListType

D = 2048
DC = 16
HD = 128
NH = 8
NE = 16
DE = 1024
EPS = 1e-6
IN_COLS = 7176
C_FQ, C_FK, C_FV, C_FL, C_RQ, C_RK, C_RV, C_RG = 0, 1024, 2048, 3072, 3080, 4104, 5128, 6152
ARENA_BYTES = 200704
import os
ATTACH_WAIT = int(os.environ.get('ATTACH_WAIT', '1'))


class Prog:
    ENGS = ("pe", "act", "dve", "pool", "sp")

    def __init__(self, nc):
        self.nc = nc
        self.ops = {e: [] for e in self.ENGS}
        self.last_w = {}
        self.readers = {}
        self.dma_cnt = {}
        self.phase = 0
        self.nphase = 1

    def _add_dep(self, op, src):
        if src is None:
            return
        if src[0] == "c":
            _, eng, idx, ph = src
            if eng == "pe" and op["eng"] == "pe":
                return
            self.ops[eng][idx]["sig"] = True
            op["cdeps"].append((eng, idx, ph))
        else:
            _, res, cnt = src
            op["dwaits"][res] = max(op["dwaits"].get(res, 0), cnt)

    def _track(self, op, ref, r, w):
        for res in r:
            self._add_dep(op, self.last_w.get(res))
        for res in w:
            self._add_dep(op, self.last_w.get(res))
            for rd in self.readers.get(res, ()):
                self._add_dep(op, rd)
        for res in r:
            self.readers.setdefault(res, []).append(ref)
        for res in w:
            self.last_w[res] = ref
            self.readers[res] = []

    def op(self, eng, fn, r=(), w=()):
        o = {"eng": eng, "fn": fn, "cdeps": [], "dwaits": {}, "sig": False, "dma": None,
             "phase": self.phase}
        idx = len(self.ops[eng])
        self.ops[eng].append(o)
        self._track(o, ("c", eng, idx, self.phase), r, w)
        return o

    def dma(self, eng, out, in_, sb, load, r=(), w=(), **kw):
        o = {"eng": eng, "fn": (lambda e: e.dma_start(out=out, in_=in_, **kw)), "cdeps": [],
             "dwaits": {}, "sig": False, "dma": sb, "phase": self.phase}
        self.ops[eng].append(o)
        cnt = self.dma_cnt.get(sb, 0) + 1
        self.dma_cnt[sb] = cnt
        ref = ("d", sb, cnt)
        if load:
            self._track(o, ref, list(r), [sb] + list(w))
        else:
            self._track(o, ref, [sb] + list(r), list(w))
        return o

    def mark(self, res, sb):
        self.last_w[res] = ("d", sb, self.dma_cnt[sb])
        self.readers[res] = []

    def barrier(self):
        lasts = {}
        for e in self.ENGS:
            for i in range(len(self.ops[e]) - 1, -1, -1):
                o = self.ops[e][i]
                if o["dma"] is None and o["fn"] is not None:
                    if o["phase"] == self.phase:
                        lasts[e] = i
                    break
        for e in self.ENGS:
            o = {"eng": e, "fn": None, "cdeps": [], "dwaits": dict(self.dma_cnt), "sig": False,
                 "dma": None, "phase": self.phase}
            for e2, i2 in lasts.items():
                if e2 != e:
                    self.ops[e2][i2]["sig"] = True
                    o["cdeps"].append((e2, i2, self.phase))
            self.ops[e].append(o)
        self.last_w = {}
        self.readers = {}
        self.phase += 1
        self.nphase = self.phase + 1

    def emit(self, final_waits=()):
        nc = self.nc
        with ExitStack() as st:
            csem = {}
            for ph in range(self.nphase):
                for e in ("pe", "act", "dve", "pool"):
                    csem[(e, ph)] = st.enter_context(nc.semaphore("c_%s_%d" % (e, ph)))

            order = list(enumerate(self.dma_cnt))
            if os.environ.get("SEMREV"):
                order = order[::-1]
            dsem = {res: st.enter_context(nc.semaphore("d_%d" % i)) for i, res in order}
            sigval = {}
            for e in self.ENGS:
                cnt = {}
                for i, o in enumerate(self.ops[e]):
                    if o["sig"]:
                        cnt[o["phase"]] = cnt.get(o["phase"], 0) + 1
                        sigval[(e, i)] = cnt[o["phase"]]
            for sm in list(csem.values()) + list(dsem.values()):
                nc.gpsimd.sem_clear(sm)
            nc.all_engine_barrier()
            block = st.enter_context(nc.Block())

            def run(eng_name):
                def body(eng):
                    seen = {}
                    for i, o in enumerate(self.ops[eng_name]):
                        waits = {}
                        for (e2, i2, ph) in o["cdeps"]:
                            k = (e2, ph)
                            waits[k] = max(waits.get(k, 0), sigval[(e2, i2)])
                        wl = []
                        for key, sm, v in ([(("c",) + k, csem[k], v) for k, v in waits.items()] +
                                           [(("d", res), dsem[res], 16 * c) for res, c in o["dwaits"].items()]):
                            if seen.get(key, 0) >= v:
                                continue
                            seen[key] = v
                            wl.append((sm, v))
                        attach = None
                        if o["fn"] is not None and wl and ATTACH_WAIT:
                            attach = wl.pop()
                        for sm, v in wl:
                            eng.wait_ge(sm, v)
                        if o["fn"] is None:
                            continue
                        ins = o["fn"](eng)
                        if attach is not None:
                            ins._wait_ge(attach[0], attach[1])
                        if o["dma"] is not None:
                            ins.then_inc(dsem[o["dma"]], 16)
                        elif o["sig"]:
                            ins.then_inc(csem[(eng_name, o["phase"])], 1)
                return body

            block.tensor(run("pe"))
            block.scalar(run("act"))
            block.vector(run("dve"))
            block.gpsimd(run("pool"))
            block.sync(run("sp"))


class Arena:
    def __init__(self, t):
        self.t = t
        self.off = 0

    def reset(self):
        self.off = 0

    def get(self, free_shape, dtype):
        n = 1
        for s in free_shape:
            n *= s
        esz = 4 if dtype == F32 else 2
        nbytes = (n * esz + 31) // 32 * 32
        assert self.off + nbytes <= ARENA_BYTES, ("arena overflow", self.off, nbytes)
        a = self.t[:, self.off // 4:(self.off + nbytes) // 4]
        self.off += nbytes
        if dtype != F32:
            a = a.bitcast(dtype)
        a = a[:, 0:n]
        if len(free_shape) == 2:
            a = a.rearrange("p (a b) -> p a b", a=free_shape[0])
        elif len(free_shape) == 3:
            a = a.rearrange("p (a b c) -> p a b c", a=free_shape[0], b=free_shape[1])
        return a


def bcast(ap2d, shape):
    return ap2d.rearrange("p (a o) -> p a o", o=1).to_broadcast(shape)


def build(NB, debug=False, upto=5):
    NQ = (NB - 1) // 2
    NT = NB * 128
    TQ = NQ * 128
    nc = bass.Bass("TRN2", target_bir_lowering=False)
    skind = "ExternalOutput" if debug else "Internal"

    def din(name, shape, dt=F32):
        return nc.dram_tensor(name, list(shape), dt, kind="ExternalInput").ap()

    def dscr(name, shape, dt=BF16):
        return nc.dram_tensor(name, list(shape), dt, kind=skind).ap()

    xin = din("xin", [NT, D])
    w_in = din("w_in", [D, IN_COLS])
    gT_attn = din("gT_attn", [128, DC])
    b_fg = din("b_fg", [1, NH])
    cst = din("cst", [128, 3 * 128 + 128])
    s_KT = dscr("s_KT", [NH * HD, NT])
    s_QT = dscr("s_QT", [NH * HD, TQ])
    s_V = dscr("s_V", [NT, 1024])
    s_RK = dscr("s_RK", [NT, 1024])
    s_RV = dscr("s_RV", [NT, 1024])
    s_RQ = dscr("s_RQ", [TQ, 1024])
    s_RG = dscr("s_RG", [TQ, 1024])
    s_OT = dscr("s_OT", [2048, TQ])
    rope_k = din("rope_k", [NT, 128])
    rope_q = din("rope_q", [TQ, 128])
    ret_MT = din("ret_MT", [128, NH * 128])
    ret_c = din("ret_c", [128, 4 * NH])
    padb = din("padb", [128, NB])
    g_fox = din("g_fox", [128, NH])
    w_out = din("w_out", [D, D])
    gT_ffn = din("gT_ffn", [128, DC])
    w_r = din("w_r", [D, 20])
    b_r = din("b_r", [1, 20])
    g_final = din("g_final", [1, D])
    w_gate = din("w_gate", [NE, D, DE])
    w_up = din("w_up", [NE, D, DE])
    w_down = din("w_down", [NE, DE, D])
    s_H = dscr("s_H", [TQ, D], F32)
    s_HT = dscr("s_HT", [D, TQ])
    out_d = nc.dram_tensor("out", [TQ, D], F32, kind="ExternalOutput").ap()
    d_comb = nc.dram_tensor("d_comb", [128, NQ * NE], F32, kind="ExternalOutput").ap() if debug else None
    d_negc = nc.dram_tensor("d_negc", [128, NB * NH], F32, kind="ExternalOutput").ap() if debug else None

    P = Prog(nc)
    with ExitStack() as st:
        arena_t = st.enter_context(nc.sbuf_tensor("arena", [128, ARENA_BYTES // 4], F32))
        A = Arena(arena_t)

        def sb(name, shape, dt):
            return st.enter_context(nc.sbuf_tensor(name, list(shape), dt))

        ident = sb("ident", [128, 128], BF16)
        Utri = sb("Utri", [128, 128], F32)
        ones = sb("ones", [128, 128], F32)
        cmask = sb("cmask", [128, 128], BF16)
        gTa = sb("gTa", [128, DC], F32)
        bfb = sb("bfb", [128, NH], F32)
        negc_all = sb("negc_all", [128, NB * NH], F32)
        endtot = sb("endtot", [128, NB * NH], F32)
        runtot = sb("runtot", [128, NH], F32)
        comb_all = sb("comb_all", [128, NQ * NE], F32)
        ssq = sb("ssq", [128, 1], F32)
        rstd = sb("rstd", [128, 1], F32)
        zt = sb("zt", [128, NH], F32)
        nl = sb("nl", [128, NH], F32)
        psb = [st.enter_context(nc.psum_tensor("ps%d" % i, [128, 512], F32)) for i in range(8)]

        P.dma("pool", ident[:, :], cst[:, 0:128], sb="ident", load=True)
        P.dma("sp", Utri[:, :], cst[:, 128:256], sb="Utri", load=True)
        P.dma("sp", ones[:, :], cst[:, 256:384], sb="ones", load=True)
        P.dma("pool", cmask[:, :], cst[:, 384:512], sb="cmask", load=True)
        P.dma("sp", gTa[:, :], gT_attn[:, :], sb="gTa", load=True)
        P.dma("sp", bfb[:, :], b_fg[0:1, :].partition_broadcast(128), sb="bfb", load=True)
        P.op("dve", lambda e: e.memset(runtot[:, :], 0.0), w=["runtot"])

        hnT = A.get([DC, NT], BF16)
        wbuf = [A.get([DC, 512], BF16) for _ in range(2)]
        xts = [A.get([D], F32) for _ in range(2)]
        hns = [A.get([D], BF16) for _ in range(2)]
        wfl = sb("wfl", [128, DC * NH], BF16)
        stg = [sb("stg%d" % i, [128, 512], BF16) for i in range(4)]
        P.dma("pool", wfl[:, :].rearrange("p (c n) -> p c n", c=DC),
              w_in[:, C_FL:C_FL + NH].rearrange("(c p) n -> p c n", p=128), sb="wfl", load=True)

        def psbf(i):
            return psb[i][:, :].bitcast(BF16)

        for L in range(NB):
            xt = xts[L % 2]
            hn = hns[L % 2]
            xr = "xt%d" % (L % 2)
            hr = "hn%d" % (L % 2)
            P.dma("sp", xt, xin[L * 128:(L + 1) * 128, :], sb=xr, load=True)
            P.op("act", lambda e, hn=hn, xt=xt: e.activation(out=hn, in_=xt, func=AF.Square,
                                                           accum_out=ssq[:, :]),
                 r=[xr], w=[hr, "ssq"])
            P.op("act", lambda e: e.activation(out=rstd[:, :], in_=ssq[:, :], func=AF.Ln,
                                               scale=1.0 / D, bias=EPS), r=["ssq"], w=["rstd"])
            P.op("act", lambda e: e.activation(out=rstd[:, :], in_=rstd[:, :], func=AF.Exp,
                                               scale=-0.5), r=["rstd"], w=["rstd"])
            P.op("act", lambda e, hn=hn, xt=xt: e.activation(out=hn, in_=xt, func=AF.Copy,
                                                           scale=rstd[:, 0:1]),
                 r=[xr, "rstd"], w=[hr])
            for cq in range(4):
                bi = cq % 2
                pr = "ps%d" % bi
                for j in range(4):
                    c = cq * 4 + j
                    P.op("pe", lambda e, bi=bi, j=j, c=c, hn=hn: e.transpose(
                        out=psbf(bi)[:, j * 128:(j + 1) * 128], in_=hn[:, c * 128:(c + 1) * 128],
                        identity=ident[:, :]), r=[hr, "ident"], w=[pr])
                P.op("dve", lambda e, bi=bi, cq=cq, L=L: e.tensor_tensor(
                    out=hnT[:, cq * 4:(cq + 1) * 4, L * 128:(L + 1) * 128],
                    in0=psbf(bi)[:, 0:512].rearrange("p (a b) -> p a b", a=4),
                    in1=bcast(gTa[:, cq * 4:(cq + 1) * 4], [128, 4, 128]), op=ALU.mult),
                    r=[pr, "gTa"], w=[("hnT", L)])
            for c in range(DC):
                P.op("pe", lambda e, c=c, L=L: e.matmul(
                    out=psb[2][:, 0:NH], lhsT=hnT[:, c, L * 128:(L + 1) * 128],
                    rhs=wfl[:, c * NH:(c + 1) * NH], start=(c == 0), stop=(c == DC - 1)),
                    r=[("hnT", L), "wfl"], w=["ps2"])
            P.op("dve", lambda e: e.tensor_tensor(out=zt[:, :], in0=psb[2][:, 0:NH], in1=bfb[:, :],
                                                  op=ALU.add), r=["ps2", "bfb"], w=["zt"])
            P.op("act", lambda e: e.activation(out=zt[:, :], in_=zt[:, :], func=AF.Exp, scale=-1.0),
                 r=["zt"], w=["zt"])
            P.op("act", lambda e: e.activation(out=nl[:, :], in_=zt[:, :], func=AF.Ln, bias=1.0),
                 r=["zt"], w=["nl"])
            P.op("pe", lambda e: e.matmul(out=psb[3][:, 0:NH], lhsT=Utri[:, :], rhs=nl[:, :],
                                          start=True, stop=True), r=["Utri", "nl"], w=["ps3"])
            P.op("pe", lambda e: e.matmul(out=psb[3][:, NH:2 * NH], lhsT=ones[:, :], rhs=nl[:, :],
                                          start=True, stop=True), r=["ones", "nl"], w=["ps3"])
            P.op("dve", lambda e, L=L: e.tensor_tensor(
                out=negc_all[:, L * NH:(L + 1) * NH], in0=psb[3][:, 0:NH], in1=runtot[:, :],
                op=ALU.add), r=["ps3", "runtot"], w=[("negc", L)])
            P.op("dve", lambda e: e.tensor_tensor(out=runtot[:, :], in0=psb[3][:, NH:2 * NH],
                                                  in1=runtot[:, :], op=ALU.add),
                 r=["ps3", "runtot"], w=["runtot"])
            P.op("dve", lambda e, L=L: e.tensor_copy(out=endtot[:, L * NH:(L + 1) * NH],
                                                     in_=runtot[:, :]),
                 r=["runtot"], w=[("endtot", L)])

        hnTq = hnT.rearrange("p c (b t) -> p c b t", t=128)
        groups = []
        for i in range(2):
            groups.append(("fm", C_FK + i * 512, s_KT, i * 512, False, 1.0))
        for i in range(2):
            groups.append(("fm", C_FQ + i * 512, s_QT, i * 512, True, HD ** -0.5))
        for i in range(2):
            groups.append(("tm", C_FV + i * 512, s_V, i * 512, False, 1.0))
        for i in range(2):
            groups.append(("tm", C_RK + i * 512, s_RK, i * 512, False, 1.0))
        for i in range(2):
            groups.append(("tm", C_RV + i * 512, s_RV, i * 512, False, 1.0))
        for i in range(2):
            groups.append(("tm", C_RQ + i * 512, s_RQ, i * 512, True, 1.0))
        for i in range(2):
            groups.append(("tm", C_RG + i * 512, s_RG, i * 512, True, 1.0))
        gemm_banks = [4, 5, 6, 7]
        nacc = 0
        for gi, (kind, c0, dst, dcol, qonly, scl) in enumerate(groups):
            wb = wbuf[gi % 2]
            wr = "wbuf%d" % (gi % 2)
            for half in range(2):
                P.dma("pool", wb[:, half * 8:(half + 1) * 8, :],
                      w_in[half * 1024:(half + 1) * 1024, c0:c0 + 512].rearrange(
                          "(c p) n -> p c n", p=128), sb=wr, load=True)
            if kind == "tm":
                blocks = list(range(2, NB, 2)) if qonly else list(range(NB))
                for bi_, L in enumerate(blocks):
                    bk = gemm_banks[nacc % 4]
                    sg = nacc % 4
                    nacc += 1
                    pr = "ps%d" % bk
                    for c in range(DC):
                        P.op("pe", lambda e, bk=bk, c=c, L=L, wb=wb: e.matmul(
                            out=psb[bk][:, :], lhsT=hnT[:, c, L * 128:(L + 1) * 128], rhs=wb[:, c, :],
                            start=(c == 0), stop=(c == DC - 1)), r=[("hnT", L), wr], w=[pr])
                    sr = "stg%d" % sg
                    if nacc % 2 == 0:
                        P.op("act", lambda e, bk=bk, sg=sg: e.activation(
                            out=stg[sg][:, :], in_=psb[bk][:, :], func=AF.Copy), r=[pr], w=[sr])
                    else:
                        P.op("dve", lambda e, bk=bk, sg=sg: e.tensor_copy(
                            out=stg[sg][:, :], in_=psb[bk][:, :]), r=[pr], w=[sr])
                    row = bi_ * 128
                    P.dma("sp", dst[row:row + 128, dcol:dcol + 512], stg[sg][:, :], sb=sr, load=False)
            else:
                for sub in range(4):
                    frow = dcol + sub * 128
                    if qonly:
                        chunks = [(q0, min(4, NQ - q0)) for q0 in range(0, NQ, 4)]
                    else:
                        chunks = [(t0, min(512, NT - t0)) for t0 in range(0, NT, 512)]
                    for (a0, an) in chunks:
                        bk = gemm_banks[nacc % 4]
                        sg = nacc % 4
                        nacc += 1
                        pr = "ps%d" % bk
                        if qonly:
                            ncol = an * 128
                            Ls = [2 + 2 * (a0 + k) for k in range(an)]
                            rres = [("hnT", L) for L in Ls]
                        else:
                            ncol = an
                            rres = [("hnT", L) for L in range(a0 // 128, (a0 + an) // 128)]
                        for c in range(DC):
                            if qonly:
                                rhs = hnTq[:, c, 2 + 2 * a0:min(NB, 2 + 2 * (a0 + an)):2, :]
                                outp = psb[bk][:, 0:ncol].rearrange("p (a t) -> p a t", t=128)
                            else:
                                rhs = hnT[:, c, a0:a0 + an]
                                outp = psb[bk][:, 0:ncol]
                            P.op("pe", lambda e, outp=outp, rhs=rhs, c=c, wb=wb, sub=sub: e.matmul(
                                out=outp, lhsT=wb[:, c, sub * 128:(sub + 1) * 128], rhs=rhs,
                                start=(c == 0), stop=(c == DC - 1)), r=rres + [wr], w=[pr])
                        sr = "stg%d" % sg
                        if nacc % 2 == 0:
                            P.op("act", lambda e, bk=bk, sg=sg, ncol=ncol, scl=scl: e.activation(
                                out=stg[sg][:, 0:ncol], in_=psb[bk][:, 0:ncol], func=AF.Copy,
                                scale=float(scl)), r=[pr], w=[sr])
                        else:
                            P.op("dve", lambda e, bk=bk, sg=sg, ncol=ncol, scl=scl: e.tensor_scalar(
                                out=stg[sg][:, 0:ncol], in0=psb[bk][:, 0:ncol], scalar1=float(scl),
                                scalar2=None, op0=ALU.mult), r=[pr], w=[sr])
                        t0 = a0 * 128 if qonly else a0
                        P.dma("sp", dst[frow:frow + 128, t0:t0 + ncol], stg[sg][:, 0:ncol], sb=sr,
                              load=False)
        if debug:
            P.dma("sp", d_negc[:, :], negc_all[:, :], sb="negc_dbg", load=False,
                  r=[("negc", L) for L in range(NB)])
        P.barrier()

        if upto >= 2:
            A.reset()
            S32 = A.get([NH, HD], F32)
            Sbf = A.get([NH, HD], BF16)
            kraw = [A.get([1024], BF16) for _ in range(2)]
            vraw = [A.get([1024], BF16) for _ in range(2)]
            qraw = [A.get([1024], BF16) for _ in range(2)]
            graw = [A.get([1024], BF16) for _ in range(2)]
            tck = [A.get([128], F32) for _ in range(2)]
            tcq = [A.get([128], F32) for _ in range(2)]
            kp_ = [A.get([1024], BF16) for _ in range(2)]
            kz_ = [A.get([1024], BF16) for _ in range(2)]
            qp_ = [A.get([1024], BF16) for _ in range(2)]
            qpp_ = [A.get([1024], BF16) for _ in range(2)]
            tmpK_ = [[A.get([NH, 64], F32) for _ in range(4)] for _ in range(2)]
            tmpQ_ = [[A.get([NH, 64], F32) for _ in range(4)] for _ in range(2)]
            kTs_ = [A.get([NH, 128], BF16) for _ in range(2)]
            qTs_ = [A.get([NH, 128], BF16) for _ in range(2)]
            smT_ = [A.get([NH, 128], BF16) for _ in range(2)]
            o32_ = [A.get([NH, HD], F32) for _ in range(2)]
            osq_ = [A.get([NH, HD], F32) for _ in range(2)]
            sgl_ = [A.get([1024], F32) for _ in range(2)]
            ybf_ = [A.get([1024], BF16) for _ in range(2)]
            oTr = [A.get([NH, 128], BF16) for _ in range(2)]
            MT = A.get([NH, 128], F32)
            rcs = A.get([4 * NH], F32)
            st1_ = [A.get([NH], F32) for _ in range(2)]
            st2_ = [A.get([NH], F32) for _ in range(2)]
            mean_ = [A.get([NH], F32) for _ in range(2)]
            rstd8_ = [A.get([NH], F32) for _ in range(2)]
            P.dma("sp", MT.rearrange("p h t -> p (h t)"), ret_MT[:, :], sb="MT", load=True)
            P.dma("sp", rcs, ret_c[:, :], sb="rcs", load=True)
            zeta = rcs[:, 0:NH]
            xi = rcs[:, NH:2 * NH]
            decay = rcs[:, 2 * NH:3 * NH]
            gret = rcs[:, 3 * NH:4 * NH]
            P.op("dve", lambda e: e.memset(S32.rearrange("p h d -> p (h d)"), 0.0), w=["S32"])
            P.op("dve", lambda e: e.memset(Sbf.rearrange("p h d -> p (h d)"), 0.0), w=["Sbf"])

            def rope(src, tab, dst, sres, tres, dres, tmp, ts):
                s4 = src.rearrange("p (h i two) -> p h i two", h=NH, two=2)
                d4 = dst.rearrange("p (h i two) -> p h i two", h=NH, two=2)
                ev, od = s4[:, :, :, 0], s4[:, :, :, 1]
                Cb = tab[:, 0:64].rearrange("p (o i) -> p o i", o=1).to_broadcast([128, NH, 64])
                Sb = tab[:, 64:128].rearrange("p (o i) -> p o i", o=1).to_broadcast([128, NH, 64])
                P.op("dve", lambda e: e.tensor_tensor(out=tmp[0], in0=ev, in1=Cb, op=ALU.mult),
                     r=[sres, tres], w=["tmp0" + ts])
                P.op("pool", lambda e: e.tensor_tensor(out=tmp[1], in0=od, in1=Sb, op=ALU.mult),
                     r=[sres, tres], w=["tmp1" + ts])
                P.op("dve", lambda e: e.tensor_tensor(out=d4[:, :, :, 0], in0=tmp[0], in1=tmp[1],
                                                      op=ALU.subtract),
                     r=["tmp0" + ts, "tmp1" + ts], w=[dres])
                P.op("pool", lambda e: e.tensor_tensor(out=tmp[2], in0=od, in1=Cb, op=ALU.mult),
                     r=[sres, tres], w=["tmp2" + ts])
                P.op("dve", lambda e: e.tensor_tensor(out=tmp[3], in0=ev, in1=Sb, op=ALU.mult),
                     r=[sres, tres], w=["tmp3" + ts])
                P.op("pool", lambda e: e.tensor_tensor(out=d4[:, :, :, 1], in0=tmp[2], in1=tmp[3],
                                                       op=ALU.add),
                     r=["tmp2" + ts, "tmp3" + ts], w=[dres])

            def bc8(a, n=HD):
                return a.rearrange("p (h o) -> p h o", o=1).to_broadcast([128, NH, n])

            def ret_block(L):
                b2 = L % 2
                kx = str(b2)
                kp, kz = kp_[b2], kz_[b2]
                KP, KZ = "kp" + kx, "kz" + kx
                isq = (L >= 2 and L % 2 == 0)
                qi = (L - 2) // 2
                kr_, vr_, tk_ = "kraw%d" % b2, "vraw%d" % b2, "tck%d" % b2
                P.dma("pool", kraw[b2], s_RK[L * 128:(L + 1) * 128, :], sb=kr_, load=True)
                P.dma("pool", vraw[b2], s_RV[L * 128:(L + 1) * 128, :], sb=vr_, load=True)
                P.dma("pool", tck[b2], rope_k[L * 128:(L + 1) * 128, :], sb=tk_, load=True)
                rope(kraw[b2], tck[b2], kp, kr_, tk_, KP, tmpK_[b2], "K" + kx)
                v3 = vraw[b2].rearrange("p (h d) -> p h d", h=NH)
                kp3 = kp.rearrange("p (h d) -> p h d", h=NH)
                kz3 = kz.rearrange("p (h d) -> p h d", h=NH)
                P.op("pool", lambda e, kp3=kp3, kz3=kz3: e.tensor_tensor(
                    out=kz3, in0=kp3, in1=bc8(zeta), op=ALU.mult), r=[KP, "rcs"], w=[KZ])
                if isq:
                    q2 = qi % 2
                    qx = str(q2)
                    qp, qpp, kTs, qTs, smT = qp_[q2], qpp_[q2], kTs_[q2], qTs_[q2], smT_[q2]
                    o32, osq, sgl, ybf = o32_[q2], osq_[q2], sgl_[q2], ybf_[q2]
                    st1, st2, mean, rstd8 = st1_[q2], st2_[q2], mean_[q2], rstd8_[q2]
                    qr_, gr_, tq_ = "qraw%d" % q2, "graw%d" % q2, "tcq%d" % q2
                    P.dma("pool", qraw[q2], s_RQ[qi * 128:(qi + 1) * 128, :], sb=qr_, load=True)
                    P.dma("pool", graw[q2], s_RG[qi * 128:(qi + 1) * 128, :], sb=gr_, load=True)
                    P.dma("pool", tcq[q2], rope_q[qi * 128:(qi + 1) * 128, :], sb=tq_, load=True)
                    rope(qraw[q2], tcq[q2], qp, qr_, tq_, "qp" + qx, tmpQ_[q2], "Q" + qx)
                    qp3 = qp.rearrange("p (h d) -> p h d", h=NH)
                    qpp3 = qpp.rearrange("p (h d) -> p h d", h=NH)
                    P.op("dve", lambda e, qp3=qp3, qpp3=qpp3: e.tensor_tensor(
                        out=qpp3, in0=qp3, in1=bc8(xi), op=ALU.mult), r=["qp" + qx, "rcs"], w=["qpp" + qx])
                    for h in range(NH):
                        P.op("pe", lambda e, h=h, kp3=kp3: e.transpose(
                            out=psbf(0)[:, h * 128:(h + 1) * 128], in_=kp3[:, h, :],
                            identity=ident[:, :]), r=[KP, "ident"], w=["ps0"])
                    P.op("act", lambda e: e.activation(
                        out=kTs.rearrange("p h t -> p (h t)"), in_=psbf(0)[:, 0:1024], func=AF.Copy),
                        r=["ps0"], w=["kTs" + qx])
                    for h in range(NH):
                        P.op("pe", lambda e, h=h, qpp3=qpp3: e.transpose(
                            out=psbf(1)[:, h * 128:(h + 1) * 128], in_=qpp3[:, h, :],
                            identity=ident[:, :]), r=["qpp" + qx, "ident"], w=["ps1"])
                    P.op("dve", lambda e: e.tensor_copy(
                        out=qTs.rearrange("p h t -> p (h t)"), in_=psbf(1)[:, 0:1024]),
                        r=["ps1"], w=["qTs" + qx])
                    for hg in range(2):
                        bk = 2 + hg
                        for hh in range(4):
                            h = hg * 4 + hh
                            P.op("pe", lambda e, h=h, hh=hh, bk=bk: e.matmul(
                                out=psb[bk][:, hh * 128:(hh + 1) * 128], lhsT=kTs[:, h, :],
                                rhs=qTs[:, h, :], start=True, stop=True),
                                r=["kTs" + qx, "qTs" + qx], w=["ps%d" % bk])
                        P.op("dve", lambda e, hg=hg, bk=bk: e.tensor_tensor(
                            out=smT[:, hg * 4:(hg + 1) * 4, :],
                            in0=psb[bk][:, :].rearrange("p (h t) -> p h t", h=4),
                            in1=MT[:, hg * 4:(hg + 1) * 4, :], op=ALU.mult),
                            r=["ps%d" % bk, "MT"], w=[("smT" + qx, hg)])
                    for hg in range(2):
                        bk = 4 + hg
                        for hh in range(4):
                            h = hg * 4 + hh
                            P.op("pe", lambda e, h=h, hh=hh, bk=bk, v3=v3: e.matmul(
                                out=psb[bk][:, hh * 128:(hh + 1) * 128], lhsT=smT[:, h, :],
                                rhs=v3[:, h, :], start=True, stop=False),
                                r=[("smT" + qx, hg), vr_], w=["ps%d" % bk])
                            P.op("pe", lambda e, h=h, hh=hh, bk=bk: e.matmul(
                                out=psb[bk][:, hh * 128:(hh + 1) * 128], lhsT=qTs[:, h, :],
                                rhs=Sbf[:, h, :], start=False, stop=True),
                                r=["qTs" + qx, "Sbf"], w=["ps%d" % bk])
                        P.op("act", lambda e, hg=hg, bk=bk: e.activation(
                            out=o32[:, hg * 4:(hg + 1) * 4, :].rearrange("p h d -> p (h d)"),
                            in_=psb[bk][:, :], func=AF.Copy), r=["ps%d" % bk], w=[("o32" + qx, hg)])
                    ores = [("o32" + qx, 0), ("o32" + qx, 1)]
                    P.op("dve", lambda e: e.tensor_reduce(out=st1, in_=o32, axis=AX.X, op=ALU.add),
                         r=ores, w=["st1" + qx])
                    P.op("act", lambda e: e.activation(
                        out=osq.rearrange("p h d -> p (h d)"), in_=o32.rearrange("p h d -> p (h d)"),
                        func=AF.Square), r=ores, w=["osq" + qx])
                    P.op("dve", lambda e: e.tensor_reduce(out=st2, in_=osq, axis=AX.X, op=ALU.add),
                         r=["osq" + qx], w=["st2" + qx])
                    P.op("dve", lambda e: e.tensor_scalar(out=mean, in0=st1, scalar1=1.0 / HD,
                                                          scalar2=None, op0=ALU.mult),
                         r=["st1" + qx], w=["mean" + qx])
                    P.op("dve", lambda e: e.tensor_tensor(out=st1, in0=mean, in1=mean, op=ALU.mult),
                         r=["mean" + qx, "st1" + qx], w=["st1" + qx])
                    P.op("dve", lambda e: e.scalar_tensor_tensor(
                        out=st2, in0=st2, scalar=1.0 / HD, in1=st1, op0=ALU.mult, op1=ALU.subtract),
                        r=["st2" + qx, "st1" + qx], w=["st2" + qx])
                    P.op("act", lambda e: e.activation(out=rstd8, in_=st2, func=AF.Ln, bias=EPS),
                         r=["st2" + qx], w=["rstd8" + qx])
                    P.op("act", lambda e: e.activation(out=rstd8, in_=rstd8, func=AF.Exp, scale=-0.5),
                         r=["rstd8" + qx], w=["rstd8" + qx])
                    P.op("dve", lambda e: e.tensor_tensor(out=o32, in0=o32, in1=bc8(mean),
                                                          op=ALU.subtract),
                         r=ores + ["mean" + qx], w=ores)
                    P.op("dve", lambda e: e.tensor_tensor(out=o32, in0=o32, in1=bc8(rstd8),
                                                          op=ALU.mult),
                         r=ores + ["rstd8" + qx], w=ores)
                    P.op("act", lambda e, q2=q2: e.activation(out=sgl, in_=graw[q2], func=AF.Silu),
                         r=[gr_], w=["sgl" + qx])
                    P.op("pool", lambda e: e.tensor_tensor(
                        out=ybf, in0=o32.rearrange("p h d -> p (h d)"), in1=sgl, op=ALU.mult),
                        r=ores + ["sgl" + qx], w=["ybf" + qx])
                    for h in range(NH):
                        P.op("pe", lambda e, h=h: e.transpose(
                            out=psbf(0)[:, h * 128:(h + 1) * 128], in_=ybf[:, h * 128:(h + 1) * 128],
                            identity=ident[:, :]), r=["ybf" + qx, "ident"], w=["ps0"])
                    ot = oTr[qi % 2]
                    otr_ = "oTr%d" % (qi % 2)
                    P.op("dve", lambda e, ot=ot: e.tensor_tensor(
                        out=ot, in0=psbf(0)[:, 0:1024].rearrange("p (h t) -> p h t", h=NH),
                        in1=bc8(gret, 128), op=ALU.mult), r=["ps0", "rcs"], w=[otr_])
                    P.dma("sp", s_OT[1024:2048, qi * 128:(qi + 1) * 128].rearrange(
                        "(c p) t -> p c t", p=128), ot, sb=otr_, load=False)
                for hg in range(2):
                    bk = 6 + hg
                    for hh in range(4):
                        h = hg * 4 + hh
                        P.op("pe", lambda e, h=h, hh=hh, bk=bk, kz3=kz3, v3=v3: e.matmul(
                            out=psb[bk][:, hh * 128:(hh + 1) * 128], lhsT=kz3[:, h, :],
                            rhs=v3[:, h, :], start=True, stop=True),
                            r=[KZ, vr_], w=["ps%d" % bk])
                P.op("dve", lambda e: e.tensor_tensor(out=S32, in0=S32, in1=bc8(decay), op=ALU.mult),
                     r=["S32", "rcs"], w=["S32"])
                for hg in range(2):
                    bk = 6 + hg
                    P.op("dve", lambda e, hg=hg, bk=bk: e.tensor_tensor(
                        out=S32[:, hg * 4:(hg + 1) * 4, :], in0=S32[:, hg * 4:(hg + 1) * 4, :],
                        in1=psb[bk][:, :].rearrange("p (h d) -> p h d", h=4), op=ALU.add),
                        r=["S32", "ps%d" % bk], w=["S32"])
                P.op("act", lambda e: e.activation(
                    out=Sbf.rearrange("p h d -> p (h d)"), in_=S32.rearrange("p h d -> p (h d)"),
                    func=AF.Copy), r=["S32"], w=["Sbf"])

            for L in range(NB):
                ret_block(L)
            P.barrier()

        if upto >= 3:
            A.reset()
            KT = A.get([NH, NT], BF16)
            Vaug = A.get([NB, NH, 130], BF16)
            qTa = [A.get([NH, 128], BF16) for _ in range(2)]
            bias = [A.get([NB, NH], F32) for _ in range(2)]
            pts = [A.get([128], BF16) for _ in range(8)]
            fo32 = A.get([NH, HD], F32)
            fjunk = A.get([1024], BF16)
            fy = A.get([1024], BF16)
            foT = [A.get([NH, 128], BF16) for _ in range(2)]
            padbt = A.get([NB], F32)
            gfx = A.get([NH], F32)
            rden = A.get([NH], F32)
            fss = A.get([1], F32)
            frs = A.get([1], F32)
            P.dma("sp", padbt, padb[:, :], sb="padbt", load=True)
            P.dma("sp", gfx, g_fox[:, :], sb="gfx", load=True)
            P.op("dve", lambda e: e.memset(Vaug[:, :, :, 128:130], 1.0),
                 w=[("V", L) for L in range(NB)])
            KCH = 11
            nch = (NB + KCH - 1) // KCH
            for j in range(nch):
                l0, l1 = j * KCH, min(NB, (j + 1) * KCH)
                P.dma("sp", KT[:, :, l0 * 128:l1 * 128],
                      s_KT[:, l0 * 128:l1 * 128].rearrange("(h p) t -> p h t", p=128),
                      sb="KTc%d" % j, load=True)
                for L in range(l0, l1):
                    P.dma("sp", Vaug[:, L, :, 0:128],
                          s_V[L * 128:(L + 1) * 128, :].rearrange("p (h d) -> p h d", h=NH),
                          sb="Vc%d" % j, load=True, w=[("V", L)])
                P.mark(("Vc", j), "Vc%d" % j)
            negc3 = negc_all[:, :].rearrange("p (b h) -> p b h", h=NH)
            obank = [(2, 0), (2, 1), (2, 2), (3, 0), (3, 1), (3, 2), (4, 0), (4, 1)]

            def oacc(h, n=129):
                bk, sl = obank[h]
                return psb[bk][:, sl * 132:sl * 132 + n]

            for qi in range(NQ):
                L = 2 + 2 * qi
                nk = L + 1
                q2 = qi % 2
                qt = qTa[q2]
                qtr = "qTa%d" % q2
                bs = bias[q2]
                bsr = "bias%d" % q2
                P.dma("sp", qt, s_QT[:, qi * 128:(qi + 1) * 128].rearrange("(h p) t -> p h t", p=128),
                      sb=qtr, load=True)
                P.op("dve", lambda e, bs=bs, nk=nk, L=L: e.tensor_tensor(
                    out=bs[:, 0:nk, :], in0=negc3[:, 0:nk, :],
                    in1=endtot[:, L * NH:(L + 1) * NH].rearrange("p (o h) -> p o h", o=1).to_broadcast(
                        [128, nk, NH]), op=ALU.subtract), w=[bsr])
                P.op("dve", lambda e, bs=bs, nk=nk: e.tensor_tensor(
                    out=bs[:, 0:nk, :], in0=bs[:, 0:nk, :],
                    in1=padbt[:, 0:nk].rearrange("p (b o) -> p b o", o=1).to_broadcast([128, nk, NH]),
                    op=ALU.add), r=["padbt", bsr], w=[bsr])
                groups_ = [(kb, hg) for kb in range(nk) for hg in range(2)]

                def st_mm(gidx, groups_=groups_, qt=qt, qtr=qtr, L=L):
                    kb, hg = groups_[gidx]
                    bk = 6 + gidx % 2
                    for hh in range(4):
                        h = hg * 4 + hh
                        diag = (kb == L)
                        P.op("pe", lambda e, h=h, hh=hh, bk=bk, kb=kb, diag=diag, qt=qt: e.matmul(
                            out=psb[bk][:, hh * 128:(hh + 1) * 128],
                            lhsT=KT[:, h, kb * 128:(kb + 1) * 128], rhs=qt[:, h, :],
                            start=True, stop=(not diag)), r=["KTc%d" % (kb // KCH), qtr], w=["ps%d" % bk])
                        if diag:
                            P.op("pe", lambda e, hh=hh, bk=bk: e.matmul(
                                out=psb[bk][:, hh * 128:(hh + 1) * 128], lhsT=ident[:, :],
                                rhs=cmask[:, :], start=False, stop=True),
                                r=["ident", "cmask"], w=["ps%d" % bk])

                def exp_pv(gidx, groups_=groups_, bs=bs, bsr=bsr, nk=nk):
                    kb, hg = groups_[gidx]
                    bk = 6 + gidx % 2
                    for hh in range(4):
                        h = hg * 4 + hh
                        slot = (gidx % 2) * 4 + hh
                        pt = pts[slot]
                        P.op("act", lambda e, pt=pt, hh=hh, bk=bk, kb=kb, h=h, bs=bs: e.activation(
                            out=pt, in_=psb[bk][:, hh * 128:(hh + 1) * 128], func=AF.Exp,
                            bias=bs[:, kb, h:h + 1], scale=1.0),
                            r=["ps%d" % bk, bsr], w=["pt%d" % slot])
                    for hh in range(4):
                        h = hg * 4 + hh
                        slot = (gidx % 2) * 4 + hh
                        pt = pts[slot]
                        first = (kb == 0 and obank[h][1] == 0)
                        P.op("pe", lambda e, pt=pt, h=h, kb=kb, first=first, nk=nk: e.matmul(
                            out=oacc(h), lhsT=pt, rhs=Vaug[:, kb, h, 0:129], start=first,
                            stop=(kb == nk - 1 and h in (2, 5, 7))), r=["pt%d" % slot, ("Vc", kb // KCH)],
                            w=["ps%d" % obank[h][0]])

                st_mm(0)
                for gidx in range(len(groups_)):
                    if gidx + 1 < len(groups_):
                        st_mm(gidx + 1)
                    exp_pv(gidx)
                for h in range(NH):
                    P.op("dve", lambda e, h=h: e.reciprocal(out=rden[:, h:h + 1], in_=oacc(h)[:, 128:129]),
                         r=["ps%d" % obank[h][0]], w=[("rden", h)])
                for h in range(NH):
                    P.op("dve", lambda e, h=h: e.tensor_scalar(
                        out=fo32[:, h, :], in0=oacc(h, 128), scalar1=rden[:, h:h + 1], scalar2=None,
                        op0=ALU.mult),
                        r=["ps%d" % obank[h][0], ("rden", h)], w=[("fo32", h)])
                fres = [("fo32", h) for h in range(NH)]
                P.op("act", lambda e: e.activation(
                    out=fjunk, in_=fo32.rearrange("p h d -> p (h d)"), func=AF.Square, accum_out=fss),
                    r=fres, w=["fjunk", "fss"])
                P.op("act", lambda e: e.activation(out=frs, in_=fss, func=AF.Ln, scale=1.0 / 1024,
                                                   bias=EPS), r=["fss"], w=["frs"])
                P.op("act", lambda e: e.activation(out=frs, in_=frs, func=AF.Exp, scale=-0.5),
                     r=["frs"], w=["frs"])
                P.op("act", lambda e: e.activation(
                    out=fy, in_=fo32.rearrange("p h d -> p (h d)"), func=AF.Copy, scale=frs[:, 0:1]),
                    r=fres + ["frs"], w=["fy"])
                for h in range(NH):
                    P.op("pe", lambda e, h=h: e.transpose(
                        out=psbf(0)[:, h * 128:(h + 1) * 128], in_=fy[:, h * 128:(h + 1) * 128],
                        identity=ident[:, :]), r=["fy", "ident"], w=["ps0"])
                ot = foT[q2]
                otr_ = "foT%d" % q2
                P.op("dve", lambda e, ot=ot: e.tensor_tensor(
                    out=ot, in0=psbf(0)[:, 0:1024].rearrange("p (h t) -> p h t", h=NH),
                    in1=gfx.rearrange("p (h o) -> p h o", o=1).to_broadcast([128, NH, 128]),
                    op=ALU.mult), r=["ps0", "gfx"], w=[otr_])
                P.dma("sp", s_OT[0:1024, qi * 128:(qi + 1) * 128].rearrange("(c p) t -> p c t", p=128),
                      ot, sb=otr_, load=False)
            P.barrier()

        if upto >= 4:
            A.reset()
            wout = A.get([DC, D], BF16)
            oTin = [A.get([DC, 128], BF16) for _ in range(2)]
            xres = [A.get([D], F32) for _ in range(2)]
            h32 = [A.get([D], F32) for _ in range(2)]
            hn2 = [A.get([D], BF16) for _ in range(2)]
            hT2 = [A.get([DC, 128], BF16) for _ in range(2)]
            wr = A.get([DC, 20], BF16)
            gTf = A.get([DC], F32)
            brb = A.get([20], F32)
            lg_all = A.get([NQ, 20], F32)
            r_g = {}
            for k in ("gmax", "sumg", "pg", "m1", "m2", "ssum", "rs", "rr"):
                r_g[k] = A.get([NQ], F32)
            for k in ("gsel", "eg", "elsel", "mask1", "el2", "sel2", "ee", "es", "wl"):
                r_g[k] = A.get([NQ, 4], F32)
            r_g["tmp16"] = A.get([NQ, 4, 4], F32)
            ss3 = A.get([1], F32)
            rs3 = A.get([1], F32)
            for cg in range(4):
                P.dma("pool", wout[:, :, cg * 512:(cg + 1) * 512],
                      w_out[:, cg * 512:(cg + 1) * 512].rearrange("(c p) n -> p c n", p=128),
                      sb="wout%d" % cg, load=True)
            P.dma("pool", wr, w_r[:, :].rearrange("(c p) n -> p c n", p=128), sb="wr", load=True)
            P.dma("sp", gTf, gT_ffn[:, :], sb="gTf", load=True)
            P.dma("sp", brb, b_r[0:1, :].partition_broadcast(128), sb="brb", load=True)

            def rop(eng, fn, r, w):
                P.op(eng, fn, r=r, w=w)

            def p3_mm(qi):
                L = 2 + 2 * qi
                b2 = qi % 2
                ot, xr_, hh_, hn_, ht_ = oTin[b2], xres[b2], h32[b2], hn2[b2], hT2[b2]
                otr, xrr, hhr, hnr, htr = ["%s%d" % (n, b2) for n in ("oTin", "xres", "h32", "hn2", "hT2")]
                P.dma("sp", ot, s_OT[:, qi * 128:(qi + 1) * 128].rearrange("(c p) t -> p c t", p=128),
                      sb=otr, load=True)
                P.dma("pool", xr_, xin[L * 128:(L + 1) * 128, :], sb=xrr, load=True)
                for cg in range(4):
                    bk = 2 + cg
                    for c in range(DC):
                        P.op("pe", lambda e, bk=bk, c=c, cg=cg, ot=ot: e.matmul(
                            out=psb[bk][:, :], lhsT=ot[:, c, :], rhs=wout[:, c, cg * 512:(cg + 1) * 512],
                            start=(c == 0), stop=(c == DC - 1)), r=[otr, "wout%d" % cg], w=["ps%d" % bk])
                    P.op("dve", lambda e, bk=bk, cg=cg, hh_=hh_, xr_=xr_: e.tensor_tensor(
                        out=hh_[:, cg * 512:(cg + 1) * 512], in0=psb[bk][:, :],
                        in1=xr_[:, cg * 512:(cg + 1) * 512], op=ALU.add),
                        r=["ps%d" % bk, xrr], w=[(hhr, cg)])
                hres = [(hhr, cg) for cg in range(4)]
                P.dma("sp", s_H[qi * 128:(qi + 1) * 128, :], hh_, sb=hhr, load=False, r=hres)

            def p3_post(qi):
                L = 2 + 2 * qi
                b2 = qi % 2
                ot, xr_, hh_, hn_, ht_ = oTin[b2], xres[b2], h32[b2], hn2[b2], hT2[b2]
                otr, xrr, hhr, hnr, htr = ["%s%d" % (n, b2) for n in ("oTin", "xres", "h32", "hn2", "hT2")]
                hres = [(hhr, cg) for cg in range(4)]
                P.op("act", lambda e, hn_=hn_, hh_=hh_: e.activation(out=hn_, in_=hh_, func=AF.Square,
                                                                   accum_out=ss3), r=hres, w=[hnr, "ss3"])
                P.op("act", lambda e: e.activation(out=rs3, in_=ss3, func=AF.Ln, scale=1.0 / D, bias=EPS),
                     r=["ss3"], w=["rs3"])
                P.op("act", lambda e: e.activation(out=rs3, in_=rs3, func=AF.Exp, scale=-0.5),
                     r=["rs3"], w=["rs3"])
                P.op("act", lambda e, hn_=hn_, hh_=hh_: e.activation(out=hn_, in_=hh_, func=AF.Copy,
                                                                   scale=rs3[:, 0:1]),
                     r=hres + ["rs3"], w=[hnr])
                for cq in range(4):
                    bi = cq % 2
                    for j in range(4):
                        c = cq * 4 + j
                        P.op("pe", lambda e, bi=bi, j=j, c=c, hn_=hn_: e.transpose(
                            out=psbf(bi)[:, j * 128:(j + 1) * 128], in_=hn_[:, c * 128:(c + 1) * 128],
                            identity=ident[:, :]), r=[hnr, "ident"], w=["ps%d" % bi])
                    P.op("dve", lambda e, bi=bi, cq=cq, ht_=ht_: e.tensor_tensor(
                        out=ht_[:, cq * 4:(cq + 1) * 4, :],
                        in0=psbf(bi)[:, 0:512].rearrange("p (a b) -> p a b", a=4),
                        in1=bcast(gTf[:, cq * 4:(cq + 1) * 4], [128, 4, 128]), op=ALU.mult),
                        r=["ps%d" % bi, "gTf"], w=[(htr, cq)])
                htres = [(htr, cq) for cq in range(4)]
                P.dma("sp", s_HT[:, qi * 128:(qi + 1) * 128].rearrange("(c p) t -> p c t", p=128), ht_,
                      sb=htr, load=False, r=htres)
                for c in range(DC):
                    P.op("pe", lambda e, c=c, ht_=ht_: e.matmul(
                        out=psb[6][:, 0:20], lhsT=ht_[:, c, :], rhs=wr[:, c, :],
                        start=(c == 0), stop=(c == DC - 1)), r=htres + ["wr"], w=["ps6"])
                P.op("dve", lambda e, qi=qi: e.tensor_tensor(out=lg_all[:, qi, :], in0=psb[6][:, 0:20], in1=brb,
                                                             op=ALU.add), r=["ps6", "brb"], w=[("lg", qi)])

            p3_mm(0)
            for qi in range(NQ):
                if qi + 1 < NQ:
                    p3_mm(qi + 1)
                p3_post(qi)
            g = r_g
            lgr = [("lg", qi) for qi in range(NQ)]
            GL = lg_all[:, :, 0:4]
            EL = lg_all[:, :, 4:20].rearrange("p q (g j) -> p q g j", g=4)

            def b3(a, n):
                return a.rearrange("p (q o) -> p q o", o=1).to_broadcast([128, NQ, n])

            P.op("dve", lambda e: e.tensor_reduce(out=g["gmax"], in_=GL, axis=AX.X, op=ALU.max),
                 r=lgr, w=["gmax"])
            P.op("dve", lambda e: e.tensor_tensor(out=g["gsel"], in0=GL, in1=b3(g["gmax"], 4), op=ALU.is_ge),
                 r=lgr + ["gmax"], w=["gsel"])
            P.op("dve", lambda e: e.tensor_tensor(out=g["eg"], in0=GL, in1=b3(g["gmax"], 4), op=ALU.subtract),
                 r=lgr + ["gmax"], w=["eg"])
            P.op("act", lambda e: e.activation(out=g["eg"], in_=g["eg"], func=AF.Exp), r=["eg"], w=["eg"])
            P.op("dve", lambda e: e.tensor_reduce(out=g["sumg"], in_=g["eg"], axis=AX.X, op=ALU.add),
                 r=["eg"], w=["sumg"])
            P.op("dve", lambda e: e.reciprocal(out=g["pg"], in_=g["sumg"]), r=["sumg"], w=["pg"])
            P.op("dve", lambda e: e.tensor_tensor(
                out=g["tmp16"], in0=EL,
                in1=g["gsel"].rearrange("p q (g o) -> p q g o", o=1).to_broadcast([128, NQ, 4, 4]),
                op=ALU.mult), r=lgr + ["gsel"], w=["tmp16"])
            P.op("dve", lambda e: e.tensor_reduce(
                out=g["elsel"], in_=g["tmp16"].rearrange("p q g j -> p q j g"), axis=AX.X, op=ALU.add),
                r=["tmp16"], w=["elsel"])
            P.op("dve", lambda e: e.tensor_reduce(out=g["m1"], in_=g["elsel"], axis=AX.X, op=ALU.max),
                 r=["elsel"], w=["m1"])
            P.op("dve", lambda e: e.tensor_tensor(out=g["mask1"], in0=g["elsel"], in1=b3(g["m1"], 4),
                                                  op=ALU.is_ge), r=["elsel", "m1"], w=["mask1"])
            P.op("dve", lambda e: e.scalar_tensor_tensor(
                out=g["el2"], in0=g["mask1"], scalar=-1e30, in1=g["elsel"], op0=ALU.mult, op1=ALU.add),
                r=["mask1", "elsel"], w=["el2"])
            P.op("dve", lambda e: e.tensor_reduce(out=g["m2"], in_=g["el2"], axis=AX.X, op=ALU.max),
                 r=["el2"], w=["m2"])
            P.op("dve", lambda e: e.tensor_tensor(out=g["sel2"], in0=g["elsel"], in1=b3(g["m2"], 4),
                                                  op=ALU.is_ge), r=["elsel", "m2"], w=["sel2"])
            P.op("dve", lambda e: e.tensor_tensor(out=g["ee"], in0=g["elsel"], in1=b3(g["m1"], 4),
                                                  op=ALU.subtract), r=["elsel", "m1"], w=["ee"])
            P.op("act", lambda e: e.activation(out=g["ee"], in_=g["ee"], func=AF.Exp), r=["ee"], w=["ee"])
            P.op("dve", lambda e: e.tensor_tensor(out=g["es"], in0=g["ee"], in1=g["sel2"], op=ALU.mult),
                 r=["ee", "sel2"], w=["es"])
            P.op("dve", lambda e: e.tensor_reduce(out=g["ssum"], in_=g["es"], axis=AX.X, op=ALU.add),
                 r=["es"], w=["ssum"])
            P.op("dve", lambda e: e.reciprocal(out=g["rs"], in_=g["ssum"]), r=["ssum"], w=["rs"])
            P.op("dve", lambda e: e.tensor_tensor(out=g["rr"], in0=g["rs"], in1=g["pg"], op=ALU.mult),
                 r=["rs", "pg"], w=["rr"])
            P.op("dve", lambda e: e.tensor_tensor(out=g["wl"], in0=g["es"], in1=b3(g["rr"], 4), op=ALU.mult),
                 r=["es", "rr"], w=["wl"])
            P.op("dve", lambda e: e.tensor_tensor(
                out=comb_all[:, :].rearrange("p (q g j) -> p q g j", g=4, j=4),
                in0=g["gsel"].rearrange("p q (g o) -> p q g o", o=1).to_broadcast([128, NQ, 4, 4]),
                in1=g["wl"].rearrange("p q (o j) -> p q o j", o=1).to_broadcast([128, NQ, 4, 4]),
                op=ALU.mult), r=["gsel", "wl"], w=[("comb", qi) for qi in range(NQ)])
            if debug:
                P.dma("sp", d_comb[:, :], comb_all[:, :], sb="comb_dbg", load=False,
                      r=[("comb", qi) for qi in range(NQ)])
            P.barrier()

        if upto >= 5:
            A.reset()
            TBH = NQ // 2
            TH = TBH * 128
            hT = A.get([DC, TH], BF16)
            yac = A.get([TBH, D], F32)
            wg = [A.get([DC, 256], BF16) for _ in range(3)]
            wu = [A.get([DC, 256], BF16) for _ in range(3)]
            wd = [A.get([2, D], BF16) for _ in range(3)]
            aTs = [A.get([2, TH], BF16) for _ in range(2)]
            sgt = [A.get([512], F32) for _ in range(2)]
            hld = A.get([D], F32)
            gfin = A.get([D], F32)
            ss4 = ssq[:, :]
            rs4 = rstd[:, :]
            P.dma("sp", gfin, g_final[0:1, :].partition_broadcast(128), sb="gfin", load=True)
            tchunks = [(t0, min(512, TH - t0)) for t0 in range(0, TH, 512)]
            groups4 = [(t0, tn, fc) for (t0, tn) in tchunks for fc in range(2)]
            cnt = {"un": 0, "npair": 0, "ndn": 0}

            def emit_down(u, tiles):
                for (tb, cg) in tiles:
                    pd_ = (0, 1, 6, 7)[cnt["ndn"] % 4]
                    cnt["ndn"] += 1
                    tcs = (tb * 128) // 512 * 512
                    aT_ = aTs[u["ab"]]
                    ares = [("aT", u["ab"], fc, tcs) for fc in range(2)]
                    for fc in range(2):
                        P.op("pe", lambda e, pd_=pd_, fc=fc, tb=tb, cg=cg, aT_=aT_, wdt=wd[u["ub"]]: e.matmul(
                            out=psb[pd_][:, :], lhsT=aT_[:, fc, tb * 128:(tb + 1) * 128],
                            rhs=wdt[:, fc, cg * 512:(cg + 1) * 512], start=(fc == 0), stop=(fc == 1)),
                            r=ares + ["wd%d" % u["ub"]], w=["ps%d" % pd_])
                    col = (u["hf"] * TBH + tb) * NE + u["ex"]
                    P.op("dve", lambda e, pd_=pd_, tb=tb, cg=cg, col=col: e.scalar_tensor_tensor(
                        out=yac[:, tb, cg * 512:(cg + 1) * 512], in0=psb[pd_][:, :],
                        scalar=comb_all[:, col:col + 1], in1=yac[:, tb, cg * 512:(cg + 1) * 512],
                        op0=ALU.mult, op1=ALU.add),
                        r=["ps%d" % pd_, ("y", tb, cg)], w=[("y", tb, cg)])

            all_tiles = [(tb, cg) for tb in range(TBH) for cg in range(4)]
            for hf in range(2):
                P.dma("sp", hT, s_HT[:, hf * TH:(hf + 1) * TH].rearrange("(c p) t -> p c t", p=128),
                      sb="hT", load=True)
                P.op("pool", lambda e: e.memset(yac.rearrange("p a b -> p (a b)"), 0.0),
                     w=[("y", tb, cg) for tb in range(TBH) for cg in range(4)])
                prev = None
                for ex in range(NE):
                    for fq in range(4):
                        ub = cnt["un"] % 3
                        ab = cnt["un"] % 2
                        cnt["un"] += 1
                        u = {"ub": ub, "ab": ab, "ex": ex, "hf": hf}
                        wgr, wur, wdr = "wg%d" % ub, "wu%d" % ub, "wd%d" % ub
                        P.dma("pool", wg[ub], w_gate[ex][:, fq * 256:(fq + 1) * 256].rearrange(
                            "(c p) f -> p c f", p=128), sb=wgr, load=True)
                        P.dma("pool", wu[ub], w_up[ex][:, fq * 256:(fq + 1) * 256].rearrange(
                            "(c p) f -> p c f", p=128), sb=wur, load=True)
                        for ch in range(2):
                            P.dma("pool", wd[ub][:, :, ch * 1024:(ch + 1) * 1024],
                                  w_down[ex][fq * 256:(fq + 1) * 256, ch * 1024:(ch + 1) * 1024].rearrange(
                                      "(j p) n -> p j n", p=128), sb=wdr, load=True)
                        ng = len(groups4)
                        for gi, (t0, tn, fc) in enumerate(groups4):
                            pg_, pu_ = (2, 3) if cnt["npair"] % 2 == 0 else (4, 5)
                            sg_ = sgt[cnt["npair"] % 2]
                            sgr = "sgt%d" % (cnt["npair"] % 2)
                            cnt["npair"] += 1
                            aT_ = aTs[ab]
                            for c in range(DC):
                                P.op("pe", lambda e, pg_=pg_, c=c, fc=fc, t0=t0, tn=tn, ub=ub: e.matmul(
                                    out=psb[pg_][:, 0:tn], lhsT=wg[ub][:, c, fc * 128:(fc + 1) * 128],
                                    rhs=hT[:, c, t0:t0 + tn], start=(c == 0), stop=(c == DC - 1)),
                                    r=[wgr, "hT"], w=["ps%d" % pg_])
                            for c in range(DC):
                                P.op("pe", lambda e, pu_=pu_, c=c, fc=fc, t0=t0, tn=tn, ub=ub: e.matmul(
                                    out=psb[pu_][:, 0:tn], lhsT=wu[ub][:, c, fc * 128:(fc + 1) * 128],
                                    rhs=hT[:, c, t0:t0 + tn], start=(c == 0), stop=(c == DC - 1)),
                                    r=[wur, "hT"], w=["ps%d" % pu_])
                            P.op("act", lambda e, pg_=pg_, sg_=sg_, tn=tn: e.activation(
                                out=sg_[:, 0:tn], in_=psb[pg_][:, 0:tn], func=AF.Silu),
                                r=["ps%d" % pg_], w=[sgr])
                            P.op("dve", lambda e, pu_=pu_, sg_=sg_, fc=fc, t0=t0, tn=tn, aT_=aT_: e.tensor_tensor(
                                out=aT_[:, fc, t0:t0 + tn], in0=sg_[:, 0:tn], in1=psb[pu_][:, 0:tn],
                                op=ALU.mult), r=[sgr, "ps%d" % pu_], w=[("aT", ab, fc, t0)])
                            if prev is not None:
                                lo = len(all_tiles) * gi // ng
                                hi = len(all_tiles) * (gi + 1) // ng
                                emit_down(prev, all_tiles[lo:hi])
                        prev = u
                emit_down(prev, all_tiles)
                for tb in range(TBH):
                    qi = hf * TBH + tb
                    yres = [("y", tb, cg) for cg in range(4)]
                    yt = yac[:, tb, :]
                    P.dma("sp", hld, s_H[qi * 128:(qi + 1) * 128, :], sb="hld", load=True)
                    P.op("dve", lambda e, yt=yt: e.tensor_tensor(out=yt, in0=yt, in1=hld, op=ALU.add),
                         r=yres + ["hld"], w=yres)
                    P.op("act", lambda e, yt=yt: e.activation(out=hld, in_=yt, func=AF.Square, accum_out=ss4),
                         r=yres, w=["hld", "ss4"])
                    P.op("act", lambda e: e.activation(out=rs4, in_=ss4, func=AF.Ln, scale=1.0 / D, bias=EPS),
                         r=["ss4"], w=["rs4"])
                    P.op("act", lambda e: e.activation(out=rs4, in_=rs4, func=AF.Exp, scale=-0.5),
                         r=["rs4"], w=["rs4"])
                    P.op("act", lambda e, yt=yt: e.activation(out=yt, in_=yt, func=AF.Copy, scale=rs4[:, 0:1]),
                         r=yres + ["rs4"], w=yres)
                    P.op("dve", lambda e, yt=yt: e.tensor_tensor(out=yt, in0=yt, in1=gfin, op=ALU.mult),
                         r=yres + ["gfin"], w=yres)
                    P.dma("sp", out_d[qi * 128:(qi + 1) * 128, :], yt, sb="yst", load=False, r=yres)
            P.barrier()

        P.emit()
    return nc


def make_consts():
    c = np.zeros((128, 512), np.float32)
    c[:, 0:128] = np.eye(128, dtype=np.float32)
    i = np.arange(128)
    c[:, 128:256] = (i[:, None] <= i[None, :]).astype(np.float32)
    c[:, 256:384] = 1.0
    c[:, 384:512] = np.where(i[None, :] >= i[:, None], 0.0, -1e30)
    return c


def ret_consts(ret_out_g):
    h = np.arange(NH, dtype=np.float32)
    gam = (1.0 - 2.0 ** (-5.0 - h)).astype(np.float64)
    t = np.arange(128, dtype=np.float64)
    zeta = gam[None, :] ** (127.0 - t[:, None])
    xi = gam[None, :] ** (t[:, None] + 1.0)
    decay = np.broadcast_to((gam ** 128.0)[None, :], (128, NH))
    gret = np.asarray(ret_out_g, np.float32).reshape(NH, 128).T
    rc = np.concatenate([zeta, xi, decay, gret], axis=1).astype(np.float32)
    j = t[:, None, None]
    i = t[None, None, :]
    MT = np.where(j <= i, gam[None, :, None] ** (-(j + 1.0)), 0.0)
    return np.ascontiguousarray(rc), np.ascontiguousarray(MT.reshape(128, NH * 128).astype(np.float32))


def rope_tables(NB, shift):
    NT = NB * 128
    pos = np.arange(NT, dtype=np.int64) - shift
    angle = (1.0 / (10000.0 ** np.linspace(0.0, 1.0, 64, dtype=np.float32))).astype(np.float32)
    phase = (pos - 112).astype(np.float32)[:, None] * angle[None, :]
    c = np.cos(phase).astype(np.float32)
    s = np.sin(phase).astype(np.float32)
    valid = (pos >= 112).astype(np.float32)[:, None]
    sc = np.float32(HD ** -0.5)
    rk = np.concatenate([c * sc * valid, s * sc * valid], axis=1).astype(np.float32)
    qrows = np.concatenate([np.arange(L * 128, (L + 1) * 128) for L in range(2, NB, 2)])
    rq = np.concatenate([c[qrows], s[qrows]], axis=1).astype(np.float32)
    padb = np.where(valid[:, 0] > 0, 0.0, -1e30).astype(np.float32).reshape(NB, 128).T
    return np.ascontiguousarray(rk), np.ascontiguousarray(rq), np.ascontiguousarray(padb)


def shared_inputs(inp):
    f = lambda a: np.ascontiguousarray(np.asarray(a, np.float32))
    sh = {
        "w_in": f(inp["w_in"][0]),
        "gT_attn": f(np.asarray(inp["attn_norm_g"][0]).reshape(DC, 128).T),
        "b_fg": f(np.asarray(inp["b_forget"][0])[None, :]),
        "cst": make_consts(),
        "g_fox": f(np.asarray(inp["fox_out_g"][0]).reshape(NH, 128).T),
        "w_out": f(inp["w_out"][0]),
        "gT_ffn": f(np.asarray(inp["ffn_norm_g"][0]).reshape(DC, 128).T),
        "w_r": f(np.concatenate([np.asarray(inp["w_router_group"][0]), np.asarray(inp["w_router_expert"][0])], axis=1)),
        "b_r": f(np.concatenate([np.asarray(inp["b_router_group"][0]), np.asarray(inp["b_router_expert"][0])])[None, :]),
        "g_final": f(np.asarray(inp["final_norm_g"])[None, :]),
        "w_gate": f(inp["w_gate"][0]),
        "w_up": f(inp["w_up"][0]),
        "w_down": f(inp["w_down"][0]),
    }
    rc, MT = ret_consts(np.asarray(inp["ret_out_g"][0]))
    sh["ret_c"] = rc
    sh["ret_MT"] = MT
    return sh


def core_inputs(x_b, meta, par, NB):
    S = (NB - 1) * 128 if par == 1 else (NB - 2) * 128
    real = np.concatenate([np.zeros((112, D), np.float32), np.asarray(meta, np.float32),
                           np.asarray(x_b[:S], np.float32)], axis=0)
    if par == 0:
        real = np.concatenate([np.zeros((128, D), np.float32), real], axis=0)
    assert real.shape[0] == NB * 128
    rk, rq, padb = rope_tables(NB, 0 if par == 1 else 128)
    return {"xin": np.ascontiguousarray(real), "rope_k": rk, "rope_q": rq, "padb": padb}


NB_FULL = 33
_NC_CACHE = {}


def kernel(**inputs):
    NB = NB_FULL
    NQ = (NB - 1) // 2
    x = np.asarray(inputs["x"], np.float32)
    B, S, _ = x.shape
    assert B * 2 == 8 and S == (NB - 1) * 128
    if "nc" not in _NC_CACHE:
        _NC_CACHE["nc"] = build(NB, debug=False, upto=5)
    nc = _NC_CACHE["nc"]
    sh = shared_inputs(inputs)
    in_maps = []
    for core in range(8):
        b, par = core // 2, core % 2
        m = dict(sh)
        m.update(core_inputs(x[b], inputs["meta_tokens"], par, NB))
        in_maps.append(m)
    res = run_bass_kernel_spmd(nc, in_maps, core_ids=list(range(8)))
    out = np.zeros((B, S, D), np.float32)
    for core in range(8):
        b, par = core // 2, core % 2
        o = np.asarray(res.results[core]["out"], np.float32)
        for qi in range(NQ):
            L = 2 + 2 * qi
            rb = L if par == 1 else L - 1
            out[b, (rb - 1) * 128:rb * 128, :] = o[qi * 128:(qi + 1) * 128, :]
    return out
```

```python
import numpy as np
import ml_dtypes
from contextlib import ExitStack
import concourse.bass as bass
import concourse.mybir as mybir
from concourse.bass_utils import run_bass_kernel_spmd

F32 = mybir.dt.float32
BF16 = mybir.dt.bfloat16
AF = mybir.ActivationFunctionType
ALU = mybir.AluOpType
AX = mybir.AxisListType

D = 2048
DC = 16
HD = 128
NH = 8
NE = 16
DE = 1024
EPS = 1e-6
IN_COLS = 7176
C_FQ, C_FK, C_FV, C_FL, C_RQ, C_RK, C_RV, C_RG = 0, 1024, 2048, 3072, 3080, 4104, 5128, 6152
ARENA_BYTES = 200704
import os
ATTACH_WAIT = int(os.environ.get('ATTACH_WAIT', '1'))


class Prog:
    ENGS = ("pe", "act", "dve", "pool", "sp")

    def __init__(self, nc):
        self.nc = nc
        self.ops = {e: [] for e in self.ENGS}
        self.last_w = {}
        self.readers = {}
        self.dma_cnt = {}
        self.phase = 0
        self.nphase = 1

    def _add_dep(self, op, src):
        if src is None:
            return
        if src[0] == "c":
            _, eng, idx, ph = src
            if eng == "pe" and op["eng"] == "pe":
                return
            self.ops[eng][idx]["sig"] = True
            op["cdeps"].append((eng, idx, ph))
        else:
            _, res, cnt = src
            op["dwaits"][res] = max(op["dwaits"].get(res, 0), cnt)

    def _track(self, op, ref, r, w):
        for res in r:
            self._add_dep(op, self.last_w.get(res))
        for res in w:
            self._add_dep(op, self.last_w.get(res))
            for rd in self.readers.get(res, ()):
                self._add_dep(op, rd)
        for res in r:
            self.readers.setdefault(res, []).append(ref)
        for res in w:
            self.last_w[res] = ref
            self.readers[res] = []

    def op(self, eng, fn, r=(), w=()):
        o = {"eng": eng, "fn": fn, "cdeps": [], "dwaits": {}, "sig": False, "dma": None,
             "phase": self.phase}
        idx = len(self.ops[eng])
        self.ops[eng].append(o)
        self._track(o, ("c", eng, idx, self.phase), r, w)
        return o

    def dma(self, eng, out, in_, sb, load, r=(), w=(), **kw):
        o = {"eng": eng, "fn": (lambda e: e.dma_start(out=out, in_=in_, **kw)), "cdeps": [],
             "dwaits": {}, "sig": False, "dma": sb, "phase": self.phase}
        self.ops[eng].append(o)
        cnt = self.dma_cnt.get(sb, 0) + 1
        self.dma_cnt[sb] = cnt
        ref = ("d", sb, cnt)
        if load:
            self._track(o, ref, list(r), [sb] + list(w))
        else:
            self._track(o, ref, [sb] + list(r), list(w))
        return o

    def mark(self, res, sb):
        self.last_w[res] = ("d", sb, self.dma_cnt[sb])
        self.readers[res] = []

    def barrier(self):
        lasts = {}
        for e in self.ENGS:
            for i in range(len(self.ops[e]) - 1, -1, -1):
                o = self.ops[e][i]
                if o["dma"] is None and o["fn"] is not None:
                    if o["phase"] == self.phase:
                        lasts[e] = i
                    break
        for e in self.ENGS:
            o = {"eng": e, "fn": None, "cdeps": [], "dwaits": dict(self.dma_cnt), "sig": False,
                 "dma": None, "phase": self.phase}
            for e2, i2 in lasts.items():
                if e2 != e:
                    self.ops[e2][i2]["sig"] = True
                    o["cdeps"].append((e2, i2, self.phase))
            self.ops[e].append(o)
        self.last_w = {}
        self.readers = {}
        self.phase += 1
        self.nphase = self.phase + 1

    def emit(self, final_waits=()):
        nc = self.nc
        with ExitStack() as st:
            csem = {}
            for ph in range(self.nphase):
                for e in ("pe", "act", "dve", "pool"):
                    csem[(e, ph)] = st.enter_context(nc.semaphore("c_%s_%d" % (e, ph)))

            order = list(enumerate(self.dma_cnt))
            if os.environ.get("SEMREV"):
                order = order[::-1]
            dsem = {res: st.enter_context(nc.semaphore("d_%d" % i)) for i, res in order}
            sigval = {}
            for e in self.ENGS:
                cnt = {}
                for i, o in enumerate(self.ops[e]):
                    if o["sig"]:
                        cnt[o["phase"]] = cnt.get(o["phase"], 0) + 1
                        sigval[(e, i)] = cnt[o["phase"]]
            for sm in list(csem.values()) + list(dsem.values()):
                nc.gpsimd.sem_clear(sm)
            nc.all_engine_barrier()
            block = st.enter_context(nc.Block())

            def run(eng_name):
                def body(eng):
                    seen = {}
                    for i, o in enumerate(self.ops[eng_name]):
                        waits = {}
                        for (e2, i2, ph) in o["cdeps"]:
                            k = (e2, ph)
                            waits[k] = max(waits.get(k, 0), sigval[(e2, i2)])
                        wl = []
                        for key, sm, v in ([(("c",) + k, csem[k], v) for k, v in waits.items()] +
                                           [(("d", res), dsem[res], 16 * c) for res, c in o["dwaits"].items()]):
                            if seen.get(key, 0) >= v:
                                continue
                            seen[key] = v
                            wl.append((sm, v))
                        attach = None
                        if o["fn"] is not None and wl and ATTACH_WAIT:
                            attach = wl.pop()
                        for sm, v in wl:
                            eng.wait_ge(sm, v)
                        if o["fn"] is None:
                            continue
                        ins = o["fn"](eng)
                        if attach is not None:
                            ins._wait_ge(attach[0], attach[1])
                        if o["dma"] is not None:
                            ins.then_inc(dsem[o["dma"]], 16)
                        elif o["sig"]:
                            ins.then_inc(csem[(eng_name, o["phase"])], 1)
                return body

            block.tensor(run("pe"))
            block.scalar(run("act"))
            block.vector(run("dve"))
            block.gpsimd(run("pool"))
            block.sync(run("sp"))


class Arena:
    def __init__(self, t):
        self.t = t
        self.off = 0

    def reset(self):
        self.off = 0

    def get(self, free_shape, dtype):
        n = 1
        for s in free_shape:
            n *= s
        esz = 4 if dtype == F32 else 2
        nbytes = (n * esz + 31) // 32 * 32
        assert self.off + nbytes <= ARENA_BYTES, ("arena overflow", self.off, nbytes)
        a = self.t[:, self.off // 4:(self.off + nbytes) // 4]
        self.off += nbytes
        if dtype != F32:
            a = a.bitcast(dtype)
        a = a[:, 0:n]
        if len(free_shape) == 2:
            a = a.rearrange("p (a b) -> p a b", a=free_shape[0])
        elif len(free_shape) == 3:
            a = a.rearrange("p (a b c) -> p a b c", a=free_shape[0], b=free_shape[1])
        return a


def bcast(ap2d, shape):
    return ap2d.rearrange("p (a o) -> p a o", o=1).to_broadcast(shape)


def build(NB, debug=False, upto=5):
    NQ = (NB - 1) // 2
    NT = NB * 128
    TQ = NQ * 128
    nc = bass.Bass("TRN2", target_bir_lowering=False)
    skind = "ExternalOutput" if debug else "Internal"

    def din(name, shape, dt=F32):
        return nc.dram_tensor(name, list(shape), dt, kind="ExternalInput").ap()

    def dscr(name, shape, dt=BF16):
        return nc.dram_tensor(name, list(shape), dt, kind=skind).ap()

    xin = din("xin", [NT, D])
    w_in = din("w_in", [D, IN_COLS])
    gT_attn = din("gT_attn", [128, DC])
    b_fg = din("b_fg", [1, NH])
    cst = din("cst", [128, 3 * 128 + 128])
    s_KT = dscr("s_KT", [NH * HD, NT])
    s_QT = dscr("s_QT", [NH * HD, TQ])
    s_V = dscr("s_V", [NT, 1024])
    s_RK = dscr("s_RK", [NT, 1024])
    s_RV = dscr("s_RV", [NT, 1024])
    s_RQ = dscr("s_RQ", [TQ, 1024])
    s_RG = dscr("s_RG", [TQ, 1024])
    s_OT = dscr("s_OT", [2048, TQ])
    rope_k = din("rope_k", [NT, 128])
    rope_q = din("rope_q", [TQ, 128])
    ret_MT = din("ret_MT", [128, NH * 128])
    ret_c = din("ret_c", [128, 4 * NH])
    padb = din("padb", [128, NB])
    g_fox = din("g_fox", [128, NH])
    w_out = din("w_out", [D, D])
    gT_ffn = din("gT_ffn", [128, DC])
    w_r = din("w_r", [D, 20])
    b_r = din("b_r", [1, 20])
    g_final = din("g_final", [1, D])
    w_gate = din("w_gate", [NE, D, DE])
    w_up = din("w_up", [NE, D, DE])
    w_down = din("w_down", [NE, DE, D])
    s_H = dscr("s_H", [TQ, D], F32)
    s_HT = dscr("s_HT", [D, TQ])
    out_d = nc.dram_tensor("out", [TQ, D], F32, kind="ExternalOutput").ap()
    d_comb = nc.dram_tensor("d_comb", [128, NQ * NE], F32, kind="ExternalOutput").ap() if debug else None
    d_negc = nc.dram_tensor("d_negc", [128, NB * NH], F32, kind="ExternalOutput").ap() if debug else None

    P = Prog(nc)
    with ExitStack() as st:
        arena_t = st.enter_context(nc.sbuf_tensor("arena", [128, ARENA_BYTES // 4], F32))
        A = Arena(arena_t)

        def sb(name, shape, dt):
            return st.enter_context(nc.sbuf_tensor(name, list(shape), dt))

        ident = sb("ident", [128, 128], BF16)
        Utri = sb("Utri", [128, 128], F32)
        ones = sb("ones", [128, 128], F32)
        cmask = sb("cmask", [128, 128], BF16)
        gTa = sb("gTa", [128, DC], F32)
        bfb = sb("bfb", [128, NH], F32)
        negc_all = sb("negc_all", [128, NB * NH], F32)
        endtot = sb("endtot", [128, NB * NH], F32)
        runtot = sb("runtot", [128, NH], F32)
        comb_all = sb("comb_all", [128, NQ * NE], F32)
        ssq = sb("ssq", [128, 1], F32)
        rstd = sb("rstd", [128, 1], F32)
        zt = sb("zt", [128, NH], F32)
        nl = sb("nl", [128, NH], F32)
        psb = [st.enter_context(nc.psum_tensor("ps%d" % i, [128, 512], F32)) for i in range(8)]

        P.dma("pool", ident[:, :], cst[:, 0:128], sb="ident", load=True)
        P.dma("sp", Utri[:, :], cst[:, 128:256], sb="Utri", load=True)
        P.dma("sp", ones[:, :], cst[:, 256:384], sb="ones", load=True)
        P.dma("pool", cmask[:, :], cst[:, 384:512], sb="cmask", load=True)
        P.dma("sp", gTa[:, :], gT_attn[:, :], sb="gTa", load=True)
        P.dma("sp", bfb[:, :], b_fg[0:1, :].partition_broadcast(128), sb="bfb", load=True)
        P.op("dve", lambda e: e.memset(runtot[:, :], 0.0), w=["runtot"])

        hnT = A.get([DC, NT], BF16)
        wbuf = [A.get([DC, 512], BF16) for _ in range(2)]
        xts = [A.get([D], F32) for _ in range(2)]
        hns = [A.get([D], BF16) for _ in range(2)]
        wfl = sb("wfl", [128, DC * NH], BF16)
        stg = [sb("stg%d" % i, [128, 512], BF16) for i in range(4)]
        P.dma("pool", wfl[:, :].rearrange("p (c n) -> p c n", c=DC),
              w_in[:, C_FL:C_FL + NH].rearrange("(c p) n -> p c n", p=128), sb="wfl", load=True)

        def psbf(i):
            return psb[i][:, :].bitcast(BF16)

        for L in range(NB):
            xt = xts[L % 2]
            hn = hns[L % 2]
            xr = "xt%d" % (L % 2)
            hr = "hn%d" % (L % 2)
            P.dma("sp", xt, xin[L * 128:(L + 1) * 128, :], sb=xr, load=True)
            P.op("act", lambda e, hn=hn, xt=xt: e.activation(out=hn, in_=xt, func=AF.Square,
                                                           accum_out=ssq[:, :]),
                 r=[xr], w=[hr, "ssq"])
            P.op("act", lambda e: e.activation(out=rstd[:, :], in_=ssq[:, :], func=AF.Ln,
                                               scale=1.0 / D, bias=EPS), r=["ssq"], w=["rstd"])
            P.op("act", lambda e: e.activation(out=rstd[:, :], in_=rstd[:, :], func=AF.Exp,
                                               scale=-0.5), r=["rstd"], w=["rstd"])
            P.op("act", lambda e, hn=hn, xt=xt: e.activation(out=hn, in_=xt, func=AF.Copy,
                                                           scale=rstd[:, 0:1]),
                 r=[xr, "rstd"], w=[hr])
            for cq in range(4):
                bi = cq % 2
                pr = "ps%d" % bi
                for j in range(4):
                    c = cq * 4 + j
                    P.op("pe", lambda e, bi=bi, j=j, c=c, hn=hn: e.transpose(
                        out=psbf(bi)[:, j * 128:(j + 1) * 128], in_=hn[:, c * 128:(c + 1) * 128],
                        identity=ident[:, :]), r=[hr, "ident"], w=[pr])
                P.op("dve", lambda e, bi=bi, cq=cq, L=L: e.tensor_tensor(
                    out=hnT[:, cq * 4:(cq + 1) * 4, L * 128:(L + 1) * 128],
                    in0=psbf(bi)[:, 0:512].rearrange("p (a b) -> p a b", a=4),
                    in1=bcast(gTa[:, cq * 4:(cq + 1) * 4], [128, 4, 128]), op=ALU.mult),
                    r=[pr, "gTa"], w=[("hnT", L)])
            for c in range(DC):
                P.op("pe", lambda e, c=c, L=L: e.matmul(
                    out=psb[2][:, 0:NH], lhsT=hnT[:, c, L * 128:(L + 1) * 128],
                    rhs=wfl[:, c * NH:(c + 1) * NH], start=(c == 0), stop=(c == DC - 1)),
                    r=[("hnT", L), "wfl"], w=["ps2"])
            P.op("dve", lambda e: e.tensor_tensor(out=zt[:, :], in0=psb[2][:, 0:NH], in1=bfb[:, :],
                                                  op=ALU.add), r=["ps2", "bfb"], w=["zt"])
            P.op("act", lambda e: e.activation(out=zt[:, :], in_=zt[:, :], func=AF.Exp, scale=-1.0),
                 r=["zt"], w=["zt"])
            P.op("act", lambda e: e.activation(out=nl[:, :], in_=zt[:, :], func=AF.Ln, bias=1.0),
                 r=["zt"], w=["nl"])
            P.op("pe", lambda e: e.matmul(out=psb[3][:, 0:NH], lhsT=Utri[:, :], rhs=nl[:, :],
                                          start=True, stop=True), r=["Utri", "nl"], w=["ps3"])
            P.op("pe", lambda e: e.matmul(out=psb[3][:, NH:2 * NH], lhsT=ones[:, :], rhs=nl[:, :],
                                          start=True, stop=True), r=["ones", "nl"], w=["ps3"])
            P.op("dve", lambda e, L=L: e.tensor_tensor(
                out=negc_all[:, L * NH:(L + 1) * NH], in0=psb[3][:, 0:NH], in1=runtot[:, :],
                op=ALU.add), r=["ps3", "runtot"], w=[("negc", L)])
            P.op("dve", lambda e: e.tensor_tensor(out=runtot[:, :], in0=psb[3][:, NH:2 * NH],
                                                  in1=runtot[:, :], op=ALU.add),
                 r=["ps3", "runtot"], w=["runtot"])
            P.op("dve", lambda e, L=L: e.tensor_copy(out=endtot[:, L * NH:(L + 1) * NH],
                                                     in_=runtot[:, :]),
                 r=["runtot"], w=[("endtot", L)])

        hnTq = hnT.rearrange("p c (b t) -> p c b t", t=128)
        groups = []
        for i in range(2):
            groups.append(("fm", C_FK + i * 512, s_KT, i * 512, False, 1.0))
        for i in range(2):
            groups.append(("fm", C_FQ + i * 512, s_QT, i * 512, True, HD ** -0.5))
        for i in range(2):
            groups.append(("tm", C_FV + i * 512, s_V, i * 512, False, 1.0))
        for i in range(2):
            groups.append(("tm", C_RK + i * 512, s_RK, i * 512, False, 1.0))
        for i in range(2):
            groups.append(("tm", C_RV + i * 512, s_RV, i * 512, False, 1.0))
        for i in range(2):
            groups.append(("tm", C_RQ + i * 512, s_RQ, i * 512, True, 1.0))
        for i in range(2):
            groups.append(("tm", C_RG + i * 512, s_RG, i * 512, True, 1.0))
        gemm_banks = [4, 5, 6, 7]
        nacc = 0
        for gi, (kind, c0, dst, dcol, qonly, scl) in enumerate(groups):
            wb = wbuf[gi % 2]
            wr = "wbuf%d" % (gi % 2)
            for half in range(2):
                P.dma("pool", wb[:, half * 8:(half + 1) * 8, :],
                      w_in[half * 1024:(half + 1) * 1024, c0:c0 + 512].rearrange(
                          "(c p) n -> p c n", p=128), sb=wr, load=True)
            if kind == "tm":
                blocks = list(range(2, NB, 2)) if qonly else list(range(NB))
                for bi_, L in enumerate(blocks):
                    bk = gemm_banks[nacc % 4]
                    sg = nacc % 4
                    nacc += 1
                    pr = "ps%d" % bk
                    for c in range(DC):
                        P.op("pe", lambda e, bk=bk, c=c, L=L, wb=wb: e.matmul(
                            out=psb[bk][:, :], lhsT=hnT[:, c, L * 128:(L + 1) * 128], rhs=wb[:, c, :],
                            start=(c == 0), stop=(c == DC - 1)), r=[("hnT", L), wr], w=[pr])
                    sr = "stg%d" % sg
                    if nacc % 2 == 0:
                        P.op("act", lambda e, bk=bk, sg=sg: e.activation(
                            out=stg[sg][:, :], in_=psb[bk][:, :], func=AF.Copy), r=[pr], w=[sr])
                    else:
                        P.op("dve", lambda e, bk=bk, sg=sg: e.tensor_copy(
                            out=stg[sg][:, :], in_=psb[bk][:, :]), r=[pr], w=[sr])
                    row = bi_ * 128
                    P.dma("sp", dst[row:row + 128, dcol:dcol + 512], stg[sg][:, :], sb=sr, load=False)
            else:
                for sub in range(4):
                    frow = dcol + sub * 128
                    if qonly:
                        chunks = [(q0, min(4, NQ - q0)) for q0 in range(0, NQ, 4)]
                    else:
                        chunks = [(t0, min(512, NT - t0)) for t0 in range(0, NT, 512)]
                    for (a0, an) in chunks:
                        bk = gemm_banks[nacc % 4]
                        sg = nacc % 4
                        nacc += 1
                        pr = "ps%d" % bk
                        if qonly:
                            ncol = an * 128
                            Ls = [2 + 2 * (a0 + k) for k in range(an)]
                            rres = [("hnT", L) for L in Ls]
                        else:
                            ncol = an
                            rres = [("hnT", L) for L in range(a0 // 128, (a0 + an) // 128)]
                        for c in range(DC):
                            if qonly:
                                rhs = hnTq[:, c, 2 + 2 * a0:min(NB, 2 + 2 * (a0 + an)):2, :]
                                outp = psb[bk][:, 0:ncol].rearrange("p (a t) -> p a t", t=128)
                            else:
                                rhs = hnT[:, c, a0:a0 + an]
                                outp = psb[bk][:, 0:ncol]
                            P.op("pe", lambda e, outp=outp, rhs=rhs, c=c, wb=wb, sub=sub: e.matmul(
                                out=outp, lhsT=wb[:, c, sub * 128:(sub + 1) * 128], rhs=rhs,
                                start=(c == 0), stop=(c == DC - 1)), r=rres + [wr], w=[pr])
                        sr = "stg%d" % sg
                        if nacc % 2 == 0:
                            P.op("act", lambda e, bk=bk, sg=sg, ncol=ncol, scl=scl: e.activation(
                                out=stg[sg][:, 0:ncol], in_=psb[bk][:, 0:ncol], func=AF.Copy,
                                scale=float(scl)), r=[pr], w=[sr])
                        else:
                            P.op("dve", lambda e, bk=bk, sg=sg, ncol=ncol, scl=scl: e.tensor_scalar(
                                out=stg[sg][:, 0:ncol], in0=psb[bk][:, 0:ncol], scalar1=float(scl),
                                scalar2=None, op0=ALU.mult), r=[pr], w=[sr])
                        t0 = a0 * 128 if qonly else a0
                        P.dma("sp", dst[frow:frow + 128, t0:t0 + ncol], stg[sg][:, 0:ncol], sb=sr,
                              load=False)
        if debug:
            P.dma("sp", d_negc[:, :], negc_all[:, :], sb="negc_dbg", load=False,
                  r=[("negc", L) for L in range(NB)])
        P.barrier()

        if upto >= 2:
            A.reset()
            S32 = A.get([NH, HD], F32)
            Sbf = A.get([NH, HD], BF16)
            kraw = [A.get([1024], BF16) for _ in range(2)]
            vraw = [A.get([1024], BF16) for _ in range(2)]
            qraw = [A.get([1024], BF16) for _ in range(2)]
            graw = [A.get([1024], BF16) for _ in range(2)]
            tck = [A.get([128], F32) for _ in range(2)]
            tcq = [A.get([128], F32) for _ in range(2)]
            kp_ = [A.get([1024], BF16) for _ in range(2)]
            kz_ = [A.get([1024], BF16) for _ in range(2)]
            qp_ = [A.get([1024], BF16) for _ in range(2)]
            qpp_ = [A.get([1024], BF16) for _ in range(2)]
            tmpK_ = [[A.get([NH, 64], F32) for _ in range(4)] for _ in range(2)]
            tmpQ_ = [[A.get([NH, 64], F32) for _ in range(4)] for _ in range(2)]
            kTs_ = [A.get([NH, 128], BF16) for _ in range(2)]
            qTs_ = [A.get([NH, 128], BF16) for _ in range(2)]
            smT_ = [A.get([NH, 128], BF16) for _ in range(2)]
            o32_ = [A.get([NH, HD], F32) for _ in range(2)]
            osq_ = [A.get([NH, HD], F32) for _ in range(2)]
            sgl_ = [A.get([1024], F32) for _ in range(2)]
            ybf_ = [A.get([1024], BF16) for _ in range(2)]
            oTr = [A.get([NH, 128], BF16) for _ in range(2)]
            MT = A.get([NH, 128], F32)
            rcs = A.get([4 * NH], F32)
            st1_ = [A.get([NH], F32) for _ in range(2)]
            st2_ = [A.get([NH], F32) for _ in range(2)]
            mean_ = [A.get([NH], F32) for _ in range(2)]
            rstd8_ = [A.get([NH], F32) for _ in range(2)]
            P.dma("sp", MT.rearrange("p h t -> p (h t)"), ret_MT[:, :], sb="MT", load=True)
            P.dma("sp", rcs, ret_c[:, :], sb="rcs", load=True)
            zeta = rcs[:, 0:NH]
            xi = rcs[:, NH:2 * NH]
            decay = rcs[:, 2 * NH:3 * NH]
            gret = rcs[:, 3 * NH:4 * NH]
            P.op("dve", lambda e: e.memset(S32.rearrange("p h d -> p (h d)"), 0.0), w=["S32"])
            P.op("dve", lambda e: e.memset(Sbf.rearrange("p h d -> p (h d)"), 0.0), w=["Sbf"])

            def rope(src, tab, dst, sres, tres, dres, tmp, ts):
                s4 = src.rearrange("p (h i two) -> p h i two", h=NH, two=2)
                d4 = dst.rearrange("p (h i two) -> p h i two", h=NH, two=2)
                ev, od = s4[:, :, :, 0], s4[:, :, :, 1]
                Cb = tab[:, 0:64].rearrange("p (o i) -> p o i", o=1).to_broadcast([128, NH, 64])
                Sb = tab[:, 64:128].rearrange("p (o i) -> p o i", o=1).to_broadcast([128, NH, 64])
                P.op("dve", lambda e: e.tensor_tensor(out=tmp[0], in0=ev, in1=Cb, op=ALU.mult),
                     r=[sres, tres], w=["tmp0" + ts])
                P.op("pool", lambda e: e.tensor_tensor(out=tmp[1], in0=od, in1=Sb, op=ALU.mult),
                     r=[sres, tres], w=["tmp1" + ts])
                P.op("dve", lambda e: e.tensor_tensor(out=d4[:, :, :, 0], in0=tmp[0], in1=tmp[1],
                                                      op=ALU.subtract),
                     r=["tmp0" + ts, "tmp1" + ts], w=[dres])
                P.op("pool", lambda e: e.tensor_tensor(out=tmp[2], in0=od, in1=Cb, op=ALU.mult),
                     r=[sres, tres], w=["tmp2" + ts])
                P.op("dve", lambda e: e.tensor_tensor(out=tmp[3], in0=ev, in1=Sb, op=ALU.mult),
                     r=[sres, tres], w=["tmp3" + ts])
                P.op("pool", lambda e: e.tensor_tensor(out=d4[:, :, :, 1], in0=tmp[2], in1=tmp[3],
                                                       op=ALU.add),
                     r=["tmp2" + ts, "tmp3" + ts], w=[dres])

            def bc8(a, n=HD):
                return a.rearrange("p (h o) -> p h o", o=1).to_broadcast([128, NH, n])

            def ret_block(L):
                b2 = L % 2
                kx = str(b2)
                kp, kz = kp_[b2], kz_[b2]
                KP, KZ = "kp" + kx, "kz" + kx
                isq = (L >= 2 and L % 2 == 0)
                qi = (L - 2) // 2
                kr_, vr_, tk_ = "kraw%d" % b2, "vraw%d" % b2, "tck%d" % b2
                P.dma("pool", kraw[b2], s_RK[L * 128:(L + 1) * 128, :], sb=kr_, load=True)
                P.dma("pool", vraw[b2], s_RV[L * 128:(L + 1) * 128, :], sb=vr_, load=True)
                P.dma("pool", tck[b2], rope_k[L * 128:(L + 1) * 128, :], sb=tk_, load=True)
                rope(kraw[b2], tck[b2], kp, kr_, tk_, KP, tmpK_[b2], "K" + kx)
                v3 = vraw[b2].rearrange("p (h d) -> p h d", h=NH)
                kp3 = kp.rearrange("p (h d) -> p h d", h=NH)
                kz3 = kz.rearrange("p (h d) -> p h d", h=NH)
                P.op("dve", lambda e, kp3=kp3, kz3=kz3: e.tensor_tensor(
                    out=kz3, in0=kp3, in1=bc8(zeta), op=ALU.mult), r=[KP, "rcs"], w=[KZ])
                if isq:
                    q2 = qi % 2
                    qx = str(q2)
                    qp, qpp, kTs, qTs, smT = qp_[q2], qpp_[q2], kTs_[q2], qTs_[q2], smT_[q2]
                    o32, osq, sgl, ybf = o32_[q2], osq_[q2], sgl_[q2], ybf_[q2]
                    st1, st2, mean, rstd8 = st1_[q2], st2_[q2], mean_[q2], rstd8_[q2]
                    qr_, gr_, tq_ = "qraw%d" % q2, "graw%d" % q2, "tcq%d" % q2
                    P.dma("pool", qraw[q2], s_RQ[qi * 128:(qi + 1) * 128, :], sb=qr_, load=True)
                    P.dma("pool", graw[q2], s_RG[qi * 128:(qi + 1) * 128, :], sb=gr_, load=True)
                    P.dma("pool", tcq[q2], rope_q[qi * 128:(qi + 1) * 128, :], sb=tq_, load=True)
                    rope(qraw[q2], tcq[q2], qp, qr_, tq_, "qp" + qx, tmpQ_[q2], "Q" + qx)
                    qp3 = qp.rearrange("p (h d) -> p h d", h=NH)
                    qpp3 = qpp.rearrange("p (h d) -> p h d", h=NH)
                    P.op("dve", lambda e, qp3=qp3, qpp3=qpp3: e.tensor_tensor(
                        out=qpp3, in0=qp3, in1=bc8(xi), op=ALU.mult), r=["qp" + qx, "rcs"], w=["qpp" + qx])
                    for h in range(NH):
                        P.op("pe", lambda e, h=h, kp3=kp3: e.transpose(
                            out=psbf(0)[:, h * 128:(h + 1) * 128], in_=kp3[:, h, :],
                            identity=ident[:, :]), r=[KP, "ident"], w=["ps0"])
                    P.op("act", lambda e: e.activation(
                        out=kTs.rearrange("p h t -> p (h t)"), in_=psbf(0)[:, 0:1024], func=AF.Copy),
                        r=["ps0"], w=["kTs" + qx])
                    for h in range(NH):
                        P.op("pe", lambda e, h=h, qpp3=qpp3: e.transpose(
                            out=psbf(1)[:, h * 128:(h + 1) * 128], in_=qpp3[:, h, :],
                            identity=ident[:, :]), r=["qpp" + qx, "ident"], w=["ps1"])
                    P.op("dve", lambda e: e.tensor_copy(
                        out=qTs.rearrange("p h t -> p (h t)"), in_=psbf(1)[:, 0:1024]),
                        r=["ps1"], w=["qTs" + qx])
                    for hg in range(2):
                        bk = 2 + hg
                        for hh in range(4):
                            h = hg * 4 + hh
                            P.op("pe", lambda e, h=h, hh=hh, bk=bk: e.matmul(
                                out=psb[bk][:, hh * 128:(hh + 1) * 128], lhsT=kTs[:, h, :],
                                rhs=qTs[:, h, :], start=True, stop=True),
                                r=["kTs" + qx, "qTs" + qx], w=["ps%d" % bk])
                        P.op("dve", lambda e, hg=hg, bk=bk: e.tensor_tensor(
                            out=smT[:, hg * 4:(hg + 1) * 4, :],
                            in0=psb[bk][:, :].rearrange("p (h t) -> p h t", h=4),
                            in1=MT[:, hg * 4:(hg + 1) * 4, :], op=ALU.mult),
                            r=["ps%d" % bk, "MT"], w=[("smT" + qx, hg)])
                    for hg in range(2):
                        bk = 4 + hg
                        for hh in range(4):
                            h = hg * 4 + hh
                            P.op("pe", lambda e, h=h, hh=hh, bk=bk, v3=v3: e.matmul(
                                out=psb[bk][:, hh * 128:(hh + 1) * 128], lhsT=smT[:, h, :],
                                rhs=v3[:, h, :], start=True, stop=False),
                                r=[("smT" + qx, hg), vr_], w=["ps%d" % bk])
                            P.op("pe", lambda e, h=h, hh=hh, bk=bk: e.matmul(
                                out=psb[bk][:, hh * 128:(hh + 1) * 128], lhsT=qTs[:, h, :],
                                rhs=Sbf[:, h, :], start=False, stop=True),
                                r=["qTs" + qx, "Sbf"], w=["ps%d" % bk])
                        P.op("act", lambda e, hg=hg, bk=bk: e.activation(
                            out=o32[:, hg * 4:(hg + 1) * 4, :].rearrange("p h d -> p (h d)"),
                            in_=psb[bk][:, :], func=AF.Copy), r=["ps%d" % bk], w=[("o32" + qx, hg)])
                    ores = [("o32" + qx, 0), ("o32" + qx, 1)]
                    P.op("dve", lambda e: e.tensor_reduce(out=st1, in_=o32, axis=AX.X, op=ALU.add),
                         r=ores, w=["st1" + qx])
                    P.op("act", lambda e: e.activation(
                        out=osq.rearrange("p h d -> p (h d)"), in_=o32.rearrange("p h d -> p (h d)"),
                        func=AF.Square), r=ores, w=["osq" + qx])
                    P.op("dve", lambda e: e.tensor_reduce(out=st2, in_=osq, axis=AX.X, op=ALU.add),
                         r=["osq" + qx], w=["st2" + qx])
                    P.op("dve", lambda e: e.tensor_scalar(out=mean, in0=st1, scalar1=1.0 / HD,
                                                          scalar2=None, op0=ALU.mult),
                         r=["st1" + qx], w=["mean" + qx])
                    P.op("dve", lambda e: e.tensor_tensor(out=st1, in0=mean, in1=mean, op=ALU.mult),
                         r=["mean" + qx, "st1" + qx], w=["st1" + qx])
                    P.op("dve", lambda e: e.scalar_tensor_tensor(
                        out=st2, in0=st2, scalar=1.0 / HD, in1=st1, op0=ALU.mult, op1=ALU.subtract),
                        r=["st2" + qx, "st1" + qx], w=["st2" + qx])
                    P.op("act", lambda e: e.activation(out=rstd8, in_=st2, func=AF.Ln, bias=EPS),
                         r=["st2" + qx], w=["rstd8" + qx])
                    P.op("act", lambda e: e.activation(out=rstd8, in_=rstd8, func=AF.Exp, scale=-0.5),
                         r=["rstd8" + qx], w=["rstd8" + qx])
                    P.op("dve", lambda e: e.tensor_tensor(out=o32, in0=o32, in1=bc8(mean),
                                                          op=ALU.subtract),
                         r=ores + ["mean" + qx], w=ores)
                    P.op("dve", lambda e: e.tensor_tensor(out=o32, in0=o32, in1=bc8(rstd8),
                                                          op=ALU.mult),
                         r=ores + ["rstd8" + qx], w=ores)
                    P.op("act", lambda e, q2=q2: e.activation(out=sgl, in_=graw[q2], func=AF.Silu),
                         r=[gr_], w=["sgl" + qx])
                    P.op("dve", lambda e: e.tensor_tensor(
                        out=ybf, in0=o32.rearrange("p h d -> p (h d)"), in1=sgl, op=ALU.mult),
                        r=ores + ["sgl" + qx], w=["ybf" + qx])
                    for h in range(NH):
                        P.op("pe", lambda e, h=h: e.transpose(
                            out=psbf(0)[:, h * 128:(h + 1) * 128], in_=ybf[:, h * 128:(h + 1) * 128],
                            identity=ident[:, :]), r=["ybf" + qx, "ident"], w=["ps0"])
                    ot = oTr[qi % 2]
                    otr_ = "oTr%d" % (qi % 2)
                    P.op("dve", lambda e, ot=ot: e.tensor_tensor(
                        out=ot, in0=psbf(0)[:, 0:1024].rearrange("p (h t) -> p h t", h=NH),
                        in1=bc8(gret, 128), op=ALU.mult), r=["ps0", "rcs"], w=[otr_])
                    P.dma("sp", s_OT[1024:2048, qi * 128:(qi + 1) * 128].rearrange(
                        "(c p) t -> p c t", p=128), ot, sb=otr_, load=False)
                for hg in range(2):
                    bk = 6 + hg
                    for hh in range(4):
                        h = hg * 4 + hh
                        P.op("pe", lambda e, h=h, hh=hh, bk=bk, kz3=kz3, v3=v3: e.matmul(
                            out=psb[bk][:, hh * 128:(hh + 1) * 128], lhsT=kz3[:, h, :],
                            rhs=v3[:, h, :], start=True, stop=True),
                            r=[KZ, vr_], w=["ps%d" % bk])
                P.op("dve", lambda e: e.tensor_tensor(out=S32, in0=S32, in1=bc8(decay), op=ALU.mult),
                     r=["S32", "rcs"], w=["S32"])
                for hg in range(2):
                    bk = 6 + hg
                    P.op("dve", lambda e, hg=hg, bk=bk: e.tensor_tensor(
                        out=S32[:, hg * 4:(hg + 1) * 4, :], in0=S32[:, hg * 4:(hg + 1) * 4, :],
                        in1=psb[bk][:, :].rearrange("p (h d) -> p h d", h=4), op=ALU.add),
                        r=["S32", "ps%d" % bk], w=["S32"])
                P.op("act", lambda e: e.activation(
                    out=Sbf.rearrange("p h d -> p (h d)"), in_=S32.rearrange("p h d -> p (h d)"),
                    func=AF.Copy), r=["S32"], w=["Sbf"])

            for L in range(NB):
                ret_block(L)
            P.barrier()

        if upto >= 3:
            A.reset()
            KT = A.get([NH, NT], BF16)
            Vaug = A.get([NB, NH, 130], BF16)
            qTa = [A.get([NH, 128], BF16) for _ in range(2)]
            bias = [A.get([NB, NH], F32) for _ in range(2)]
            pts = [A.get([128], BF16) for _ in range(12)]
            fo32 = A.get([NH, HD], F32)
            fjunk = A.get([1024], BF16)
            fy = A.get([1024], BF16)
            foT = [A.get([NH, 128], BF16) for _ in range(2)]
            padbt = A.get([NB], F32)
            gfx = A.get([NH], F32)
            rden = A.get([NH], F32)
            fss = A.get([1], F32)
            frs = A.get([1], F32)
            P.dma("sp", padbt, padb[:, :], sb="padbt", load=True)
            P.dma("sp", gfx, g_fox[:, :], sb="gfx", load=True)
            P.op("dve", lambda e: e.memset(Vaug[:, :, :, 128:130], 1.0),
                 w=[("V", L) for L in range(NB)])
            KCH = 11
            nch = (NB + KCH - 1) // KCH
            for j in range(nch):
                l0, l1 = j * KCH, min(NB, (j + 1) * KCH)
                P.dma("sp", KT[:, :, l0 * 128:l1 * 128],
                      s_KT[:, l0 * 128:l1 * 128].rearrange("(h p) t -> p h t", p=128),
                      sb="KTc%d" % j, load=True)
                for L in range(l0, l1):
                    P.dma("sp", Vaug[:, L, :, 0:128],
                          s_V[L * 128:(L + 1) * 128, :].rearrange("p (h d) -> p h d", h=NH),
                          sb="Vc%d" % j, load=True, w=[("V", L)])
                P.mark(("Vc", j), "Vc%d" % j)
            negc3 = negc_all[:, :].rearrange("p (b h) -> p b h", h=NH)
            obank = [(2, 0), (2, 1), (2, 2), (3, 0), (3, 1), (3, 2), (4, 0), (4, 1)]

            def oacc(h, n=129):
                bk, sl = obank[h]
                return psb[bk][:, sl * 132:sl * 132 + n]

            for qi in range(NQ):
                L = 2 + 2 * qi
                nk = L + 1
                q2 = qi % 2
                qt = qTa[q2]
                qtr = "qTa%d" % q2
                bs = bias[q2]
                bsr = "bias%d" % q2
                P.dma("sp", qt, s_QT[:, qi * 128:(qi + 1) * 128].rearrange("(h p) t -> p h t", p=128),
                      sb=qtr, load=True)
                P.op("dve", lambda e, bs=bs, nk=nk, L=L: e.tensor_tensor(
                    out=bs[:, 0:nk, :], in0=negc3[:, 0:nk, :],
                    in1=endtot[:, L * NH:(L + 1) * NH].rearrange("p (o h) -> p o h", o=1).to_broadcast(
                        [128, nk, NH]), op=ALU.subtract), w=[bsr])
                P.op("dve", lambda e, bs=bs, nk=nk: e.tensor_tensor(
                    out=bs[:, 0:nk, :], in0=bs[:, 0:nk, :],
                    in1=padbt[:, 0:nk].rearrange("p (b o) -> p b o", o=1).to_broadcast([128, nk, NH]),
                    op=ALU.add), r=["padbt", bsr], w=[bsr])
                groups_ = [(kb, hg) for kb in range(nk) for hg in range(2)]

                def st_mm(gidx, groups_=groups_, qt=qt, qtr=qtr, L=L):
                    kb, hg = groups_[gidx]
                    bk = (6, 7, 5)[gidx % 3]
                    for hh in range(4):
                        h = hg * 4 + hh
                        diag = (kb == L)
                        P.op("pe", lambda e, h=h, hh=hh, bk=bk, kb=kb, diag=diag, qt=qt: e.matmul(
                            out=psb[bk][:, hh * 128:(hh + 1) * 128],
                            lhsT=KT[:, h, kb * 128:(kb + 1) * 128], rhs=qt[:, h, :],
                            start=True, stop=(not diag)), r=["KTc%d" % (kb // KCH), qtr], w=["ps%d" % bk])
                        if diag:
                            P.op("pe", lambda e, hh=hh, bk=bk: e.matmul(
                                out=psb[bk][:, hh * 128:(hh + 1) * 128], lhsT=ident[:, :],
                                rhs=cmask[:, :], start=False, stop=True),
                                r=["ident", "cmask"], w=["ps%d" % bk])

                def exp_pv(gidx, groups_=groups_, bs=bs, bsr=bsr, nk=nk):
                    kb, hg = groups_[gidx]
                    bk = (6, 7, 5)[gidx % 3]
                    for hh in range(4):
                        h = hg * 4 + hh
                        slot = (gidx % 3) * 4 + hh
                        pt = pts[slot]
                        P.op("act", lambda e, pt=pt, hh=hh, bk=bk, kb=kb, h=h, bs=bs: e.activation(
                            out=pt, in_=psb[bk][:, hh * 128:(hh + 1) * 128], func=AF.Exp,
                            bias=bs[:, kb, h:h + 1], scale=1.0),
                            r=["ps%d" % bk, bsr], w=["pt%d" % slot])
                    for hh in range(4):
                        h = hg * 4 + hh
                        slot = (gidx % 3) * 4 + hh
                        pt = pts[slot]
                        first = (kb == 0 and obank[h][1] == 0)
                        P.op("pe", lambda e, pt=pt, h=h, kb=kb, first=first, nk=nk: e.matmul(
                            out=oacc(h), lhsT=pt, rhs=Vaug[:, kb, h, 0:129], start=first,
                            stop=(kb == nk - 1 and h in (2, 5, 7))), r=["pt%d" % slot, ("Vc", kb // KCH)],
                            w=["ps%d" % obank[h][0]])

                st_mm(0)
                if len(groups_) > 1:
                    st_mm(1)
                for gidx in range(len(groups_)):
                    if gidx + 2 < len(groups_):
                        st_mm(gidx + 2)
                    exp_pv(gidx)
                for h in range(NH):
                    P.op("dve", lambda e, h=h: e.reciprocal(out=rden[:, h:h + 1], in_=oacc(h)[:, 128:129]),
                         r=["ps%d" % obank[h][0]], w=[("rden", h)])
                for h in range(NH):
                    P.op("dve", lambda e, h=h: e.tensor_scalar(
                        out=fo32[:, h, :], in0=oacc(h, 128), scalar1=rden[:, h:h + 1], scalar2=None,
                        op0=ALU.mult),
                        r=["ps%d" % obank[h][0], ("rden", h)], w=[("fo32", h)])
                fres = [("fo32", h) for h in range(NH)]
                P.op("act", lambda e: e.activation(
                    out=fjunk, in_=fo32.rearrange("p h d -> p (h d)"), func=AF.Square, accum_out=fss),
                    r=fres, w=["fjunk", "fss"])
                P.op("act", lambda e: e.activation(out=frs, in_=fss, func=AF.Ln, scale=1.0 / 1024,
                                                   bias=EPS), r=["fss"], w=["frs"])
                P.op("act", lambda e: e.activation(out=frs, in_=frs, func=AF.Exp, scale=-0.5),
                     r=["frs"], w=["frs"])
                P.op("act", lambda e: e.activation(
                    out=fy, in_=fo32.rearrange("p h d -> p (h d)"), func=AF.Copy, scale=frs[:, 0:1]),
                    r=fres + ["frs"], w=["fy"])
                for h in range(NH):
                    P.op("pe", lambda e, h=h: e.transpose(
                        out=psbf(0)[:, h * 128:(h + 1) * 128], in_=fy[:, h * 128:(h + 1) * 128],
                        identity=ident[:, :]), r=["fy", "ident"], w=["ps0"])
                ot = foT[q2]
                otr_ = "foT%d" % q2
                P.op("dve", lambda e, ot=ot: e.tensor_tensor(
                    out=ot, in0=psbf(0)[:, 0:1024].rearrange("p (h t) -> p h t", h=NH),
                    in1=gfx.rearrange("p (h o) -> p h o", o=1).to_broadcast([128, NH, 128]),
                    op=ALU.mult), r=["ps0", "gfx"], w=[otr_])
                P.dma("sp", s_OT[0:1024, qi * 128:(qi + 1) * 128].rearrange("(c p) t -> p c t", p=128),
                      ot, sb=otr_, load=False)
            P.barrier()

        if upto >= 4:
            A.reset()
            wout = A.get([DC, D], BF16)
            oTin = [A.get([DC, 128], BF16) for _ in range(2)]
            xres = [A.get([D], F32) for _ in range(2)]
            h32 = [A.get([D], F32) for _ in range(2)]
            hn2 = [A.get([D], BF16) for _ in range(2)]
            hT2 = [A.get([DC, 128], BF16) for _ in range(2)]
            wr = A.get([DC, 20], BF16)
            gTf = A.get([DC], F32)
            brb = A.get([20], F32)
            lg_all = A.get([NQ, 20], F32)
            r_g = {}
            for k in ("gmax", "sumg", "pg", "m1", "m2", "ssum", "rs", "rr"):
                r_g[k] = A.get([NQ], F32)
            for k in ("gsel", "eg", "elsel", "mask1", "el2", "sel2", "ee", "es", "wl"):
                r_g[k] = A.get([NQ, 4], F32)
            r_g["tmp16"] = A.get([NQ, 4, 4], F32)
            ss3 = A.get([1], F32)
            rs3 = A.get([1], F32)
            for cg in range(4):
                P.dma("pool", wout[:, :, cg * 512:(cg + 1) * 512],
                      w_out[:, cg * 512:(cg + 1) * 512].rearrange("(c p) n -> p c n", p=128),
                      sb="wout%d" % cg, load=True)
            P.dma("pool", wr, w_r[:, :].rearrange("(c p) n -> p c n", p=128), sb="wr", load=True)
            P.dma("sp", gTf, gT_ffn[:, :], sb="gTf", load=True)
            P.dma("sp", brb, b_r[0:1, :].partition_broadcast(128), sb="brb", load=True)

            def rop(eng, fn, r, w):
                P.op(eng, fn, r=r, w=w)

            def p3_mm(qi):
                L = 2 + 2 * qi
                b2 = qi % 2
                ot, xr_, hh_, hn_, ht_ = oTin[b2], xres[b2], h32[b2], hn2[b2], hT2[b2]
                otr, xrr, hhr, hnr, htr = ["%s%d" % (n, b2) for n in ("oTin", "xres", "h32", "hn2", "hT2")]
                P.dma("sp", ot, s_OT[:, qi * 128:(qi + 1) * 128].rearrange("(c p) t -> p c t", p=128),
                      sb=otr, load=True)
                P.dma("pool", xr_, xin[L * 128:(L + 1) * 128, :], sb=xrr, load=True)
                for cg in range(4):
                    bk = 2 + cg
                    for c in range(DC):
                        P.op("pe", lambda e, bk=bk, c=c, cg=cg, ot=ot: e.matmul(
                            out=psb[bk][:, :], lhsT=ot[:, c, :], rhs=wout[:, c, cg * 512:(cg + 1) * 512],
                            start=(c == 0), stop=(c == DC - 1)), r=[otr, "wout%d" % cg], w=["ps%d" % bk])
                    P.op("dve", lambda e, bk=bk, cg=cg, hh_=hh_, xr_=xr_: e.tensor_tensor(
                        out=hh_[:, cg * 512:(cg + 1) * 512], in0=psb[bk][:, :],
                        in1=xr_[:, cg * 512:(cg + 1) * 512], op=ALU.add),
                        r=["ps%d" % bk, xrr], w=[(hhr, cg)])
                hres = [(hhr, cg) for cg in range(4)]
                P.dma("sp", s_H[qi * 128:(qi + 1) * 128, :], hh_, sb=hhr, load=False, r=hres)

            def p3_post(qi):
                L = 2 + 2 * qi
                b2 = qi % 2
                ot, xr_, hh_, hn_, ht_ = oTin[b2], xres[b2], h32[b2], hn2[b2], hT2[b2]
                otr, xrr, hhr, hnr, htr = ["%s%d" % (n, b2) for n in ("oTin", "xres", "h32", "hn2", "hT2")]
                hres = [(hhr, cg) for cg in range(4)]
                P.op("act", lambda e, hn_=hn_, hh_=hh_: e.activation(out=hn_, in_=hh_, func=AF.Square,
                                                                   accum_out=ss3), r=hres, w=[hnr, "ss3"])
                P.op("act", lambda e: e.activation(out=rs3, in_=ss3, func=AF.Ln, scale=1.0 / D, bias=EPS),
                     r=["ss3"], w=["rs3"])
                P.op("act", lambda e: e.activation(out=rs3, in_=rs3, func=AF.Exp, scale=-0.5),
                     r=["rs3"], w=["rs3"])
                P.op("act", lambda e, hn_=hn_, hh_=hh_: e.activation(out=hn_, in_=hh_, func=AF.Copy,
                                                                   scale=rs3[:, 0:1]),
                     r=hres + ["rs3"], w=[hnr])
                for cq in range(4):
                    bi = cq % 2
                    for j in range(4):
                        c = cq * 4 + j
                        P.op("pe", lambda e, bi=bi, j=j, c=c, hn_=hn_: e.transpose(
                            out=psbf(bi)[:, j * 128:(j + 1) * 128], in_=hn_[:, c * 128:(c + 1) * 128],
                            identity=ident[:, :]), r=[hnr, "ident"], w=["ps%d" % bi])
                    P.op("dve", lambda e, bi=bi, cq=cq, ht_=ht_: e.tensor_tensor(
                        out=ht_[:, cq * 4:(cq + 1) * 4, :],
                        in0=psbf(bi)[:, 0:512].rearrange("p (a b) -> p a b", a=4),
                        in1=bcast(gTf[:, cq * 4:(cq + 1) * 4], [128, 4, 128]), op=ALU.mult),
                        r=["ps%d" % bi, "gTf"], w=[(htr, cq)])
                htres = [(htr, cq) for cq in range(4)]
                P.dma("sp", s_HT[:, qi * 128:(qi + 1) * 128].rearrange("(c p) t -> p c t", p=128), ht_,
                      sb=htr, load=False, r=htres)
                for c in range(DC):
                    P.op("pe", lambda e, c=c, ht_=ht_: e.matmul(
                        out=psb[6][:, 0:20], lhsT=ht_[:, c, :], rhs=wr[:, c, :],
                        start=(c == 0), stop=(c == DC - 1)), r=htres + ["wr"], w=["ps6"])
                P.op("dve", lambda e, qi=qi: e.tensor_tensor(out=lg_all[:, qi, :], in0=psb[6][:, 0:20], in1=brb,
                                                             op=ALU.add), r=["ps6", "brb"], w=[("lg", qi)])

            p3_mm(0)
            for qi in range(NQ):
                if qi + 1 < NQ:
                    p3_mm(qi + 1)
                p3_post(qi)
            g = r_g
            lgr = [("lg", qi) for qi in range(NQ)]
            GL = lg_all[:, :, 0:4]
            EL = lg_all[:, :, 4:20].rearrange("p q (g j) -> p q g j", g=4)

            def b3(a, n):
                return a.rearrange("p (q o) -> p q o", o=1).to_broadcast([128, NQ, n])

            P.op("dve", lambda e: e.tensor_reduce(out=g["gmax"], in_=GL, axis=AX.X, op=ALU.max),
                 r=lgr, w=["gmax"])
            P.op("dve", lambda e: e.tensor_tensor(out=g["gsel"], in0=GL, in1=b3(g["gmax"], 4), op=ALU.is_ge),
                 r=lgr + ["gmax"], w=["gsel"])
            P.op("dve", lambda e: e.tensor_tensor(out=g["eg"], in0=GL, in1=b3(g["gmax"], 4), op=ALU.subtract),
                 r=lgr + ["gmax"], w=["eg"])
            P.op("act", lambda e: e.activation(out=g["eg"], in_=g["eg"], func=AF.Exp), r=["eg"], w=["eg"])
            P.op("dve", lambda e: e.tensor_reduce(out=g["sumg"], in_=g["eg"], axis=AX.X, op=ALU.add),
                 r=["eg"], w=["sumg"])
            P.op("dve", lambda e: e.reciprocal(out=g["pg"], in_=g["sumg"]), r=["sumg"], w=["pg"])
            P.op("dve", lambda e: e.tensor_tensor(
                out=g["tmp16"], in0=EL,
                in1=g["gsel"].rearrange("p q (g o) -> p q g o", o=1).to_broadcast([128, NQ, 4, 4]),
                op=ALU.mult), r=lgr + ["gsel"], w=["tmp16"])
            P.op("dve", lambda e: e.tensor_reduce(
                out=g["elsel"], in_=g["tmp16"].rearrange("p q g j -> p q j g"), axis=AX.X, op=ALU.add),
                r=["tmp16"], w=["elsel"])
            P.op("dve", lambda e: e.tensor_reduce(out=g["m1"], in_=g["elsel"], axis=AX.X, op=ALU.max),
                 r=["elsel"], w=["m1"])
            P.op("dve", lambda e: e.tensor_tensor(out=g["mask1"], in0=g["elsel"], in1=b3(g["m1"], 4),
                                                  op=ALU.is_ge), r=["elsel", "m1"], w=["mask1"])
            P.op("dve", lambda e: e.scalar_tensor_tensor(
                out=g["el2"], in0=g["mask1"], scalar=-1e30, in1=g["elsel"], op0=ALU.mult, op1=ALU.add),
                r=["mask1", "elsel"], w=["el2"])
            P.op("dve", lambda e: e.tensor_reduce(out=g["m2"], in_=g["el2"], axis=AX.X, op=ALU.max),
                 r=["el2"], w=["m2"])
            P.op("dve", lambda e: e.tensor_tensor(out=g["sel2"], in0=g["elsel"], in1=b3(g["m2"], 4),
                                                  op=ALU.is_ge), r=["elsel", "m2"], w=["sel2"])
            P.op("dve", lambda e: e.tensor_tensor(out=g["ee"], in0=g["elsel"], in1=b3(g["m1"], 4),
                                                  op=ALU.subtract), r=["elsel", "m1"], w=["ee"])
            P.op("act", lambda e: e.activation(out=g["ee"], in_=g["ee"], func=AF.Exp), r=["ee"], w=["ee"])
            P.op("dve", lambda e: e.tensor_tensor(out=g["es"], in0=g["ee"], in1=g["sel2"], op=ALU.mult),
                 r=["ee", "sel2"], w=["es"])
            P.op("dve", lambda e: e.tensor_reduce(out=g["ssum"], in_=g["es"], axis=AX.X, op=ALU.add),
                 r=["es"], w=["ssum"])
            P.op("dve", lambda e: e.reciprocal(out=g["rs"], in_=g["ssum"]), r=["ssum"], w=["rs"])
            P.op("dve", lambda e: e.tensor_tensor(out=g["rr"], in0=g["rs"], in1=g["pg"], op=ALU.mult),
                 r=["rs", "pg"], w=["rr"])
            P.op("dve", lambda e: e.tensor_tensor(out=g["wl"], in0=g["es"], in1=b3(g["rr"], 4), op=ALU.mult),
                 r=["es", "rr"], w=["wl"])
            P.op("dve", lambda e: e.tensor_tensor(
                out=comb_all[:, :].rearrange("p (q g j) -> p q g j", g=4, j=4),
                in0=g["gsel"].rearrange("p q (g o) -> p q g o", o=1).to_broadcast([128, NQ, 4, 4]),
                in1=g["wl"].rearrange("p q (o j) -> p q o j", o=1).to_broadcast([128, NQ, 4, 4]),
                op=ALU.mult), r=["gsel", "wl"], w=[("comb", qi) for qi in range(NQ)])
            if debug:
                P.dma("sp", d_comb[:, :], comb_all[:, :], sb="comb_dbg", load=False,
                      r=[("comb", qi) for qi in range(NQ)])
            P.barrier()

        if upto >= 5:
            A.reset()
            TBH = NQ // 2
            TH = TBH * 128
            hT = A.get([DC, TH], BF16)
            yac = A.get([TBH, D], F32)
            wg = [A.get([DC, 256], BF16) for _ in range(3)]
            wu = [A.get([DC, 256], BF16) for _ in range(3)]
            wd = [A.get([2, D], BF16) for _ in range(3)]
            aTs = [A.get([2, TH], BF16) for _ in range(2)]
            sgt = [A.get([512], F32) for _ in range(2)]
            hld = A.get([D], F32)
            gfin = A.get([D], F32)
            ss4 = ssq[:, :]
            rs4 = rstd[:, :]
            P.dma("sp", gfin, g_final[0:1, :].partition_broadcast(128), sb="gfin", load=True)
            tchunks = [(t0, min(512, TH - t0)) for t0 in range(0, TH, 512)]
            groups4 = [(t0, tn, fc) for (t0, tn) in tchunks for fc in range(2)]
            cnt = {"un": 0, "npair": 0, "ndn": 0}

            def emit_down(u, tiles):
                for (tb, cg) in tiles:
                    pd_ = (0, 1, 6, 7)[cnt["ndn"] % 4]
                    cnt["ndn"] += 1
                    tcs = (tb * 128) // 512 * 512
                    aT_ = aTs[u["ab"]]
                    ares = [("aT", u["ab"], fc, tcs) for fc in range(2)]
                    for fc in range(2):
                        P.op("pe", lambda e, pd_=pd_, fc=fc, tb=tb, cg=cg, aT_=aT_, wdt=wd[u["ub"]]: e.matmul(
                            out=psb[pd_][:, :], lhsT=aT_[:, fc, tb * 128:(tb + 1) * 128],
                            rhs=wdt[:, fc, cg * 512:(cg + 1) * 512], start=(fc == 0), stop=(fc == 1)),
                            r=ares + ["wd%d" % u["ub"]], w=["ps%d" % pd_])
                    col = (u["hf"] * TBH + tb) * NE + u["ex"]
                    P.op("dve", lambda e, pd_=pd_, tb=tb, cg=cg, col=col: e.scalar_tensor_tensor(
                        out=yac[:, tb, cg * 512:(cg + 1) * 512], in0=psb[pd_][:, :],
                        scalar=comb_all[:, col:col + 1], in1=yac[:, tb, cg * 512:(cg + 1) * 512],
                        op0=ALU.mult, op1=ALU.add),
                        r=["ps%d" % pd_, ("y", tb, cg)], w=[("y", tb, cg)])

            all_tiles = [(tb, cg) for tb in range(TBH) for cg in range(4)]
            for hf in range(2):
                P.dma("sp", hT, s_HT[:, hf * TH:(hf + 1) * TH].rearrange("(c p) t -> p c t", p=128),
                      sb="hT", load=True)
                P.op("pool", lambda e: e.memset(yac.rearrange("p a b -> p (a b)"), 0.0),
                     w=[("y", tb, cg) for tb in range(TBH) for cg in range(4)])
                prev = None
                for ex in range(NE):
                    for fq in range(4):
                        ub = cnt["un"] % 3
                        ab = cnt["un"] % 2
                        cnt["un"] += 1
                        u = {"ub": ub, "ab": ab, "ex": ex, "hf": hf}
                        wgr, wur, wdr = "wg%d" % ub, "wu%d" % ub, "wd%d" % ub
                        P.dma("pool", wg[ub], w_gate[ex][:, fq * 256:(fq + 1) * 256].rearrange(
                            "(c p) f -> p c f", p=128), sb=wgr, load=True)
                        P.dma("pool", wu[ub], w_up[ex][:, fq * 256:(fq + 1) * 256].rearrange(
                            "(c p) f -> p c f", p=128), sb=wur, load=True)
                        for ch in range(2):
                            P.dma("pool", wd[ub][:, :, ch * 1024:(ch + 1) * 1024],
                                  w_down[ex][fq * 256:(fq + 1) * 256, ch * 1024:(ch + 1) * 1024].rearrange(
                                      "(j p) n -> p j n", p=128), sb=wdr, load=True)
                        ng = len(groups4)
                        for gi, (t0, tn, fc) in enumerate(groups4):
                            pg_, pu_ = (2, 3) if cnt["npair"] % 2 == 0 else (4, 5)
                            sg_ = sgt[cnt["npair"] % 2]
                            sgr = "sgt%d" % (cnt["npair"] % 2)
                            cnt["npair"] += 1
                            aT_ = aTs[ab]
                            for c in range(DC):
                                P.op("pe", lambda e, pg_=pg_, c=c, fc=fc, t0=t0, tn=tn, ub=ub: e.matmul(
                                    out=psb[pg_][:, 0:tn], lhsT=wg[ub][:, c, fc * 128:(fc + 1) * 128],
                                    rhs=hT[:, c, t0:t0 + tn], start=(c == 0), stop=(c == DC - 1)),
                                    r=[wgr, "hT"], w=["ps%d" % pg_])
                            for c in range(DC):
                                P.op("pe", lambda e, pu_=pu_, c=c, fc=fc, t0=t0, tn=tn, ub=ub: e.matmul(
                                    out=psb[pu_][:, 0:tn], lhsT=wu[ub][:, c, fc * 128:(fc + 1) * 128],
                                    rhs=hT[:, c, t0:t0 + tn], start=(c == 0), stop=(c == DC - 1)),
                                    r=[wur, "hT"], w=["ps%d" % pu_])
                            P.op("act", lambda e, pg_=pg_, sg_=sg_, tn=tn: e.activation(
                                out=sg_[:, 0:tn], in_=psb[pg_][:, 0:tn], func=AF.Silu),
                                r=["ps%d" % pg_], w=[sgr])
                            P.op("dve", lambda e, pu_=pu_, sg_=sg_, fc=fc, t0=t0, tn=tn, aT_=aT_: e.tensor_tensor(
                                out=aT_[:, fc, t0:t0 + tn], in0=sg_[:, 0:tn], in1=psb[pu_][:, 0:tn],
                                op=ALU.mult), r=[sgr, "ps%d" % pu_], w=[("aT", ab, fc, t0)])
                            if prev is not None:
                                lo = len(all_tiles) * gi // ng
                                hi = len(all_tiles) * (gi + 1) // ng
                                emit_down(prev, all_tiles[lo:hi])
                        prev = u
                emit_down(prev, all_tiles)
                for tb in range(TBH):
                    qi = hf * TBH + tb
                    yres = [("y", tb, cg) for cg in range(4)]
                    yt = yac[:, tb, :]
                    P.dma("sp", hld, s_H[qi * 128:(qi + 1) * 128, :], sb="hld", load=True)
                    P.op("dve", lambda e, yt=yt: e.tensor_tensor(out=yt, in0=yt, in1=hld, op=ALU.add),
                         r=yres + ["hld"], w=yres)
                    P.op("act", lambda e, yt=yt: e.activation(out=hld, in_=yt, func=AF.Square, accum_out=ss4),
                         r=yres, w=["hld", "ss4"])
                    P.op("act", lambda e: e.activation(out=rs4, in_=ss4, func=AF.Ln, scale=1.0 / D, bias=EPS),
                         r=["ss4"], w=["rs4"])
                    P.op("act", lambda e: e.activation(out=rs4, in_=rs4, func=AF.Exp, scale=-0.5),
                         r=["rs4"], w=["rs4"])
                    P.op("act", lambda e, yt=yt: e.activation(out=yt, in_=yt, func=AF.Copy, scale=rs4[:, 0:1]),
                         r=yres + ["rs4"], w=yres)
                    P.op("dve", lambda e, yt=yt: e.tensor_tensor(out=yt, in0=yt, in1=gfin, op=ALU.mult),
                         r=yres + ["gfin"], w=yres)
                    P.dma("sp", out_d[qi * 128:(qi + 1) * 128, :], yt, sb="yst", load=False, r=yres)
            P.barrier()

        P.emit()
    return nc


def make_consts():
    c = np.zeros((128, 512), np.float32)
    c[:, 0:128] = np.eye(128, dtype=np.float32)
    i = np.arange(128)
    c[:, 128:256] = (i[:, None] <= i[None, :]).astype(np.float32)
    c[:, 256:384] = 1.0
    c[:, 384:512] = np.where(i[None, :] >= i[:, None], 0.0, -1e30)
    return c


def ret_consts(ret_out_g):
    h = np.arange(NH, dtype=np.float32)
    gam = (1.0 - 2.0 ** (-5.0 - h)).astype(np.float64)
    t = np.arange(128, dtype=np.float64)
    zeta = gam[None, :] ** (127.0 - t[:, None])
    xi = gam[None, :] ** (t[:, None] + 1.0)
    decay = np.broadcast_to((gam ** 128.0)[None, :], (128, NH))
    gret = np.asarray(ret_out_g, np.float32).reshape(NH, 128).T
    rc = np.concatenate([zeta, xi, decay, gret], axis=1).astype(np.float32)
    j = t[:, None, None]
    i = t[None, None, :]
    MT = np.where(j <= i, gam[None, :, None] ** (-(j + 1.0)), 0.0)
    return np.ascontiguousarray(rc), np.ascontiguousarray(MT.reshape(128, NH * 128).astype(np.float32))


def rope_tables(NB, shift):
    NT = NB * 128
    pos = np.arange(NT, dtype=np.int64) - shift
    angle = (1.0 / (10000.0 ** np.linspace(0.0, 1.0, 64, dtype=np.float32))).astype(np.float32)
    phase = (pos - 112).astype(np.float32)[:, None] * angle[None, :]
    c = np.cos(phase).astype(np.float32)
    s = np.sin(phase).astype(np.float32)
    valid = (pos >= 112).astype(np.float32)[:, None]
    sc = np.float32(HD ** -0.5)
    rk = np.concatenate([c * sc * valid, s * sc * valid], axis=1).astype(np.float32)
    qrows = np.concatenate([np.arange(L * 128, (L + 1) * 128) for L in range(2, NB, 2)])
    rq = np.concatenate([c[qrows], s[qrows]], axis=1).astype(np.float32)
    padb = np.where(valid[:, 0] > 0, 0.0, -1e30).astype(np.float32).reshape(NB, 128).T
    return np.ascontiguousarray(rk), np.ascontiguousarray(rq), np.ascontiguousarray(padb)


def shared_inputs(inp):
    f = lambda a: np.ascontiguousarray(np.asarray(a, np.float32))
    sh = {
        "w_in": f(inp["w_in"][0]),
        "gT_attn": f(np.asarray(inp["attn_norm_g"][0]).reshape(DC, 128).T),
        "b_fg": f(np.asarray(inp["b_forget"][0])[None, :]),
        "cst": make_consts(),
        "g_fox": f(np.asarray(inp["fox_out_g"][0]).reshape(NH, 128).T),
        "w_out": f(inp["w_out"][0]),
        "gT_ffn": f(np.asarray(inp["ffn_norm_g"][0]).reshape(DC, 128).T),
        "w_r": f(np.concatenate([np.asarray(inp["w_router_group"][0]), np.asarray(inp["w_router_expert"][0])], axis=1)),
        "b_r": f(np.concatenate([np.asarray(inp["b_router_group"][0]), np.asarray(inp["b_router_expert"][0])])[None, :]),
        "g_final": f(np.asarray(inp["final_norm_g"])[None, :]),
        "w_gate": f(inp["w_gate"][0]),
        "w_up": f(inp["w_up"][0]),
        "w_down": f(inp["w_down"][0]),
    }
    rc, MT = ret_consts(np.asarray(inp["ret_out_g"][0]))
    sh["ret_c"] = rc
    sh["ret_MT"] = MT
    return sh


def core_inputs(x_b, meta, par, NB):
    S = (NB - 1) * 128 if par == 1 else (NB - 2) * 128
    real = np.concatenate([np.zeros((112, D), np.float32), np.asarray(meta, np.float32),
                           np.asarray(x_b[:S], np.float32)], axis=0)
    if par == 0:
        real = np.concatenate([np.zeros((128, D), np.float32), real], axis=0)
    assert real.shape[0] == NB * 128
    rk, rq, padb = rope_tables(NB, 0 if par == 1 else 128)
    return {"xin": np.ascontiguousarray(real), "rope_k": rk, "rope_q": rq, "padb": padb}


NB_FULL = 33
_NC_CACHE = {}


def kernel(**inputs):
    NB = NB_FULL
    NQ = (NB - 1) // 2
    x = np.asarray(inputs["x"], np.float32)
    B, S, _ = x.shape
    assert B * 2 == 8 and S == (NB - 1) * 128
    if "nc" not in _NC_CACHE:
        _NC_CACHE["nc"] = build(NB, debug=False, upto=5)
    nc = _NC_CACHE["nc"]
    sh = shared_inputs(inputs)
    in_maps = []
    for core in range(8):
        b, par = core // 2, core % 2
        m = dict(sh)
        m.update(core_inputs(x[b], inputs["meta_tokens"], par, NB))
        in_maps.append(m)
    res = run_bass_kernel_spmd(nc, in_maps, core_ids=list(range(8)))
    out = np.zeros((B, S, D), np.float32)
    for core in range(8):
        b, par = core // 2, core % 2
        o = np.asarray(res.results[core]["out"], np.float32)
        for qi in range(NQ):
            L = 2 + 2 * qi
            rb = L if par == 1 else L - 1
            out[b, (rb - 1) * 128:rb * 128, :] = o[qi * 128:(qi + 1) * 128, :]
    return out
```
